# Optimizing a Trainium2 kernel written in Bass

```python
import jax, jax.numpy as jnp
from jax import lax
import numpy as np

D_MODEL = 2048
BATCH = 16
SEQ = 2048
DEPTH = 4

CTX_LEN = 256
GRID_W = 64
RMS_EPS = 1e-6
N_MOD = 6
ROPE_THETA = 10000.0
GLA_HEADS = 4
GLA_DK = D_MODEL // 16
GLA_DV = D_MODEL // 8
GLA_KEY_WIDTH = GLA_HEADS * GLA_DK
GLA_VAL_WIDTH = GLA_HEADS * GLA_DV
GLA_RANK = 16
GLA_TAU = 16.0
GLA_CHUNK = 64
LRU_WIDTH = D_MODEL // 2
LRU_BLOCKS = 8
LRU_BLOCK_DIM = LRU_WIDTH // LRU_BLOCKS
LRU_CONV = 4
LRU_C = 8.0
REC_SPLITS = [GLA_KEY_WIDTH, 2 * GLA_KEY_WIDTH, 2 * GLA_KEY_WIDTH + GLA_VAL_WIDTH, 2 * GLA_KEY_WIDTH + 2 * GLA_VAL_WIDTH, 2 * GLA_KEY_WIDTH + 2 * GLA_VAL_WIDTH + GLA_RANK, 2 * GLA_KEY_WIDTH + 2 * GLA_VAL_WIDTH + 2 * GLA_RANK, 2 * GLA_KEY_WIDTH + 2 * GLA_VAL_WIDTH + 2 * GLA_RANK + LRU_WIDTH]
REC_IN = 2 * GLA_KEY_WIDTH + 2 * GLA_VAL_WIDTH + 2 * GLA_RANK + 2 * LRU_WIDTH
NA_HEADS = 16
NA_HEAD_DIM = D_MODEL // NA_HEADS
NA_KH = 8
NA_KW = 16
NA_QBLOCK_W = 16
NA_BAND_W = 32
N_EXPERTS = 16
EC_CAPACITY_FACTOR = 2
EXPERT_FF = D_MODEL // 2
N_REC_LAYERS = (DEPTH + 1) // 2
N_NA_LAYERS = DEPTH // 2

kernel_name = "hybrid_gla_rglru_natten_ec_dit"


def rms_norm(x, g):
    xf = x.astype(jnp.float32)
    y = xf * lax.rsqrt(jnp.mean(xf * xf, axis=-1, keepdims=True) + RMS_EPS)
    return (y * g.astype(jnp.float32)).astype(x.dtype)


def modulate(h, shift, scale):
    return h * (1 + scale) + shift


def axial_rope(n_tok, head_dim):
    t = jnp.arange(n_tok)
    rows = (t // GRID_W).astype(jnp.float32)
    cols = (t % GRID_W).astype(jnp.float32)
    n_freq = head_dim // 4
    inv = ROPE_THETA ** (-jnp.arange(n_freq, dtype=jnp.float32) / n_freq)
    ang = jnp.concatenate([rows[:, None] * inv, cols[:, None] * inv], axis=-1)
    return jnp.cos(ang), jnp.sin(ang)


def apply_rope(t, cos, sin):
    tp = t.astype(jnp.float32).reshape(*t.shape[:-1], -1, 2)
    t1, t2 = tp[..., 0], tp[..., 1]
    out = jnp.stack([t1 * cos - t2 * sin, t1 * sin + t2 * cos], axis=-1)
    return out.reshape(t.shape).astype(t.dtype)


def gla_chunked(q, k, v, log_a, s0):
    B, H, T, DK = q.shape
    DV = v.shape[-1]
    n = T // GLA_CHUNK

    def chunks(t):
        return t.astype(jnp.float32).reshape(B, H, n, GLA_CHUNK, t.shape[-1])

    q, k, v, la = chunks(q), chunks(k), chunks(v), chunks(log_a)
    b = jnp.cumsum(la, axis=3)
    b_end = b[:, :, :, -1:, :]
    q_dec = q * jnp.exp(b)
    k_inv = k * jnp.exp(-b)
    k_end = k * jnp.exp(b_end - b)
    lower = np.tril(np.ones((GLA_CHUNK, GLA_CHUNK), dtype=bool))
    scores = jnp.where(lower, jnp.einsum('bhnid,bhnjd->bhnij', q_dec, k_inv), 0.0)
    o_intra = jnp.einsum('bhnij,bhnjv->bhniv', scores, v)

    def step(state, xs):
        q_c, k_c, v_c, dec = xs
        o_c = jnp.einsum('bhid,bhdv->bhiv', q_c, state)
        state = state * dec[..., None] + jnp.einsum('bhjd,bhjv->bhdv', k_c, v_c)
        return state, o_c

    xs = (jnp.moveaxis(q_dec, 2, 0), jnp.moveaxis(k_end, 2, 0), jnp.moveaxis(v, 2, 0),
          jnp.moveaxis(jnp.exp(b_end[:, :, :, 0, :]), 2, 0))
    s_fin, o_inter = lax.scan(step, s0.astype(jnp.float32), xs)
    o = o_intra + jnp.moveaxis(o_inter, 0, 2)
    return o.reshape(B, H, T, DV), s_fin


def gla_bidir(q, k, v, la_f, la_b, s_f, s_b):
    o_f, fin_f = gla_chunked(q, k, v, la_f, s_f)
    o_b, fin_b = gla_chunked(q[:, :, ::-1], k[:, :, ::-1], v[:, :, ::-1], la_b[:, :, ::-1], s_b)
    return o_f + o_b[:, :, ::-1], fin_f, fin_b


def gla_inputs(p, alpha_up, alpha_b, cos, sin):
    q, k, v, _, za_f, za_b = p[:6]
    B, T, _ = q.shape

    def heads(t, d):
        return t.reshape(B, T, GLA_HEADS, d).transpose(0, 2, 1, 3)

    q = heads(q, GLA_DK) * GLA_DK ** -0.5
    k = heads(k, GLA_DK)
    v = heads(v, GLA_DV)
    if cos is not None:
        q, k = apply_rope(q, cos, sin), apply_rope(k, cos, sin)
    la_f = heads(jax.nn.log_sigmoid((za_f @ alpha_up[0] + alpha_b[0]).astype(jnp.float32)) / GLA_TAU, GLA_DK)
    la_b = heads(jax.nn.log_sigmoid((za_b @ alpha_up[1] + alpha_b[1]).astype(jnp.float32)) / GLA_TAU, GLA_DK)
    return q, k, v, la_f, la_b


def gla_output(o, g, gla_g):
    B, H, T, DV = o.shape
    o = o * lax.rsqrt(jnp.mean(o * o, axis=-1, keepdims=True) + RMS_EPS) * gla_g.astype(jnp.float32)
    o = o.transpose(0, 2, 1, 3).reshape(B, T, H * DV)
    return o * jax.nn.silu(g.astype(jnp.float32))


def depthwise_conv_centred(x, w, b):
    K, W = w.shape
    lo = (K - 1) // 2
    y = lax.conv_general_dilated(x, w.astype(x.dtype)[:, None, :], window_strides=(1,), padding=[(lo, K - 1 - lo)],
                                 dimension_numbers=('NWC', 'WIO', 'NWC'), feature_group_count=W)
    return y + b


def block_diag_linear(x, w, b):
    xb = x.reshape(*x.shape[:-1], LRU_BLOCKS, LRU_BLOCK_DIM)
    return jnp.einsum('...nd,nde->...ne', xb, w).reshape(x.shape) + b


def linear_scan(a, bx, h0, reverse):
    if reverse:
        a, bx = a[:, ::-1], bx[:, ::-1]
    bx = bx.at[:, 0].add(a[:, 0] * h0)

    def comb(left, right):
        return left[0] * right[0], right[0] * left[1] + right[1]

    _, h = lax.associative_scan(comb, (a, bx), axis=1)
    h_fin = h[:, -1]
    if reverse:
        h = h[:, ::-1]
    return h, h_fin


def rglru_direction(xc, w_a, b_a, w_i, b_i, lam, h0, reverse):
    r = jax.nn.sigmoid(block_diag_linear(xc, w_a, b_a))
    i = jax.nn.sigmoid(block_diag_linear(xc, w_i, b_i))
    log_a = -LRU_C * r * jax.nn.softplus(-lam.astype(jnp.float32))
    a = jnp.exp(log_a)
    bx = jnp.sqrt(-jnp.expm1(2.0 * log_a)) * (i * xc)
    return linear_scan(a, bx, h0, reverse)


def rglru_bidir(xc, w_a, b_a, w_i, b_i, lam, h_f, h_b):
    o_f, fin_f = rglru_direction(xc, w_a[0], b_a[0], w_i[0], b_i[0], lam[0], h_f, False)
    o_b, fin_b = rglru_direction(xc, w_a[1], b_a[1], w_i[1], b_i[1], lam[1], h_b, True)
    return o_f + o_b, fin_f, fin_b


def rec_mixer(h_ctx, h_lat, w_in, alpha_up, alpha_b, gla_g, conv_w, conv_b, w_a, b_a, w_i, b_i, lam, cos, sin, need_ctx_out):
    p_ctx = jnp.split(h_ctx @ w_in, REC_SPLITS, axis=-1)
    p_lat = jnp.split(h_lat @ w_in, REC_SPLITS, axis=-1)
    B = h_lat.shape[0]
    qc, kc, vc, lac_f, lac_b = gla_inputs(p_ctx, alpha_up, alpha_b, None, None)
    ql, kl, vl, lal_f, lal_b = gla_inputs(p_lat, alpha_up, alpha_b, cos, sin)
    s0 = jnp.zeros((B, GLA_HEADS, GLA_DK, GLA_DV), jnp.float32)
    oc, sc_f, sc_b = gla_bidir(qc, kc, vc, lac_f, lac_b, s0, s0)
    ol, _, _ = gla_bidir(ql, kl, vl, lal_f, lal_b, sc_f, sc_b)
    xc_ctx = depthwise_conv_centred(p_ctx[6].astype(jnp.float32), conv_w, conv_b)
    xc_lat = depthwise_conv_centred(p_lat[6].astype(jnp.float32), conv_w, conv_b)
    h0 = jnp.zeros((B, LRU_WIDTH), jnp.float32)
    hc, hc_f, hc_b = rglru_bidir(xc_ctx, w_a, b_a, w_i, b_i, lam, h0, h0)
    hl, _, _ = rglru_bidir(xc_lat, w_a, b_a, w_i, b_i, lam, hc_f, hc_b)
    y_lat = jnp.concatenate([gla_output(ol, p_lat[3], gla_g), hl * jax.nn.gelu(p_lat[7].astype(jnp.float32))], axis=-1).astype(h_lat.dtype)
    if not need_ctx_out:
        return None, y_lat
    y_ctx = jnp.concatenate([gla_output(oc, p_ctx[3], gla_g), hc * jax.nn.gelu(p_ctx[7].astype(jnp.float32))], axis=-1).astype(h_ctx.dtype)
    return y_ctx, y_lat


def na_mixer(h_ctx, h_lat, w_qkv, rpb, need_ctx_out):
    B, T, _ = h_lat.shape
    rows = T // GRID_W
    kh = min(NA_KH, rows)

    def split_heads(h):
        qkv = (h @ w_qkv).reshape(B, h.shape[1], 3, NA_HEADS, NA_HEAD_DIM)
        return qkv[:, :, 0].transpose(0, 2, 1, 3), qkv[:, :, 1].transpose(0, 2, 1, 3), qkv[:, :, 2].transpose(0, 2, 1, 3)

    q_l, k_l, v_l = split_heads(h_lat)
    q_c, k_c, v_c = split_heads(h_ctx)
    scale = NA_HEAD_DIM ** -0.5
    k_g = k_l.reshape(B, NA_HEADS, rows, GRID_W, NA_HEAD_DIM)
    v_g = v_l.reshape(B, NA_HEADS, rows, GRID_W, NA_HEAD_DIM)
    q_rows = jnp.moveaxis(q_l.reshape(B, NA_HEADS, rows, GRID_W, NA_HEAD_DIM), 2, 0)
    r = np.arange(rows)
    row_start = np.clip(r - kh // 2, 0, rows - kh)
    dr = row_start[:, None] + np.arange(kh)[None, :] - r[:, None]
    n_qb = GRID_W // NA_QBLOCK_W
    qcol = np.arange(GRID_W).reshape(n_qb, NA_QBLOCK_W)
    band_start = np.clip(np.arange(n_qb) * NA_QBLOCK_W - NA_KW // 2, 0, GRID_W - NA_BAND_W)
    band_cols = band_start[:, None] + np.arange(NA_BAND_W)[None, :]
    col_start = np.clip(qcol - NA_KW // 2, 0, GRID_W - NA_KW)
    col_valid = (band_cols[:, None, :] >= col_start[..., None]) & (band_cols[:, None, :] < col_start[..., None] + NA_KW)
    dc_idx = np.clip(band_cols[:, None, :] - qcol[..., None], -(NA_KW - 1), NA_KW - 1) + NA_KW - 1
    key_mask = np.broadcast_to(col_valid[:, :, None, :], (n_qb, NA_QBLOCK_W, kh, NA_BAND_W)).reshape(n_qb, NA_QBLOCK_W, kh * NA_BAND_W)
    rpb_c = rpb[:, :, dc_idx]
    n_loc = kh * NA_BAND_W

    def band(t):
        t = t[:, :, :, band_cols]
        return t.transpose(0, 1, 3, 2, 4, 5).reshape(B, NA_HEADS, n_qb, n_loc, NA_HEAD_DIM)

    def row_block(args):
        q_r, rs, dr_r = args
        kb = band(lax.dynamic_slice_in_dim(k_g, rs, kh, axis=2))
        vb = band(lax.dynamic_slice_in_dim(v_g, rs, kh, axis=2))
        qb = q_r.reshape(B, NA_HEADS, n_qb, NA_QBLOCK_W, NA_HEAD_DIM)
        bias = rpb_c[:, dr_r + NA_KH - 1].transpose(0, 2, 3, 1, 4).reshape(NA_HEADS, n_qb, NA_QBLOCK_W, n_loc)
        bias = jnp.where(key_mask, bias.astype(jnp.float32), -jnp.inf)
        s_loc = jnp.einsum('bhjqd,bhjkd->bhjqk', qb, kb).astype(jnp.float32) * scale + bias
        s_ctx = jnp.einsum('bhjqd,bhld->bhjql', qb, k_c).astype(jnp.float32) * scale
        p = jax.nn.softmax(jnp.concatenate([s_loc, s_ctx], axis=-1), axis=-1).astype(v_g.dtype)
        o = jnp.einsum('bhjqk,bhjkd->bhjqd', p[..., :n_loc], vb) + jnp.einsum('bhjql,bhld->bhjqd', p[..., n_loc:], v_c)
        return o.reshape(B, NA_HEADS, GRID_W, NA_HEAD_DIM)

    o_rows = lax.map(row_block, (q_rows, jnp.asarray(row_start, jnp.int32), jnp.asarray(dr, jnp.int32)))
    y_lat = o_rows.transpose(1, 0, 3, 2, 4).reshape(B, T, D_MODEL)
    if not need_ctx_out:
        return None, y_lat
    s = jnp.einsum('bhqd,bhkd->bhqk', q_c, k_c).astype(jnp.float32) * scale
    o_c = jnp.einsum('bhqk,bhkd->bhqd', jax.nn.softmax(s, axis=-1).astype(v_c.dtype), v_c)
    y_ctx = o_c.transpose(0, 2, 1, 3).reshape(B, h_ctx.shape[1], D_MODEL)
    return y_ctx, y_lat


def expert_choice_ffn(h, w_r, w_g, w_u, w_d):
    B, N, _ = h.shape
    cap = EC_CAPACITY_FACTOR * N // N_EXPERTS
    aff = jax.nn.softmax((h @ w_r).astype(jnp.float32), axis=-1)
    gate, idx = lax.top_k(jnp.swapaxes(aff, 1, 2), cap)
    bidx = jnp.arange(B)[:, None, None]
    xs = h[bidx, idx]
    hid = jax.nn.silu(jnp.einsum('becd,edf->becf', xs, w_g)) * jnp.einsum('becd,edf->becf', xs, w_u)
    y = jnp.einsum('becf,efd->becd', hid, w_d) * gate[..., None].astype(h.dtype)
    return jnp.zeros_like(h).at[bidx, idx].add(y)


def setup_inputs(seed: int = 0) -> dict:
    key = jax.random.key(seed)
    ks = iter(jax.random.split(key, 40))
    f32 = jnp.float32

    def nrm(shape, scale):
        return scale * jax.random.normal(next(ks), shape, f32)

    D, E, F = D_MODEL, N_EXPERTS, EXPERT_FF
    L, R, A = DEPTH, N_REC_LAYERS, N_NA_LAYERS
    u = jax.random.uniform(next(ks), (R, 2, LRU_WIDTH), f32, 0.9, 0.999)
    a0 = u ** (1.0 / LRU_C)
    lam = jnp.log(a0) - jnp.log1p(-a0)
    return {
        'x': nrm((BATCH, SEQ, D), 1.0),
        'c': nrm((BATCH, D), 1.0),
        'ctx': nrm((BATCH, CTX_LEN, D), 1.0),
        'c_ctx': nrm((D,), 1.0),
        'w_mod': nrm((L, D, N_MOD * D), 0.5 * D ** -0.5),
        'b_mod': nrm((L, N_MOD * D), 0.01),
        'norm_mix_g': 1.0 + nrm((L, D), 0.05),
        'norm_ffn_g': 1.0 + nrm((L, D), 0.05),
        'w_out': nrm((L, D, D), D ** -0.5),
        'router_w': nrm((L, D, E), D ** -0.5),
        'exp_w_gate': nrm((L, E, D, F), D ** -0.5),
        'exp_w_up': nrm((L, E, D, F), D ** -0.5),
        'exp_w_down': nrm((L, E, F, D), F ** -0.5),
        'rec_w_in': nrm((R, D, REC_IN), D ** -0.5),
        'gla_alpha_up': nrm((R, 2, GLA_RANK, GLA_KEY_WIDTH), GLA_RANK ** -0.5),
        'gla_alpha_b': nrm((R, 2, GLA_KEY_WIDTH), 0.1),
        'gla_norm_g': 1.0 + nrm((R, GLA_DV), 0.05),
        'lru_conv_w': nrm((R, LRU_CONV, LRU_WIDTH), LRU_CONV ** -0.5),
        'lru_conv_b': nrm((R, LRU_WIDTH), 0.01),
        'lru_w_a': nrm((R, 2, LRU_BLOCKS, LRU_BLOCK_DIM, LRU_BLOCK_DIM), LRU_BLOCK_DIM ** -0.5),
        'lru_b_a': nrm((R, 2, LRU_WIDTH), 0.01),
        'lru_w_i': nrm((R, 2, LRU_BLOCKS, LRU_BLOCK_DIM, LRU_BLOCK_DIM), LRU_BLOCK_DIM ** -0.5),
        'lru_b_i': nrm((R, 2, LRU_WIDTH), 0.01),
        'lru_lambda': lam,
        'na_w_qkv': nrm((A, D, 3 * D), D ** -0.5),
        'na_rpb': nrm((A, NA_HEADS, 2 * NA_KH - 1, 2 * NA_KW - 1), 0.02),
        'norm_f_g': 1.0 + nrm((D,), 0.05),
    }


def reference(x, c, ctx, c_ctx, w_mod, b_mod, norm_mix_g, norm_ffn_g, w_out, router_w, exp_w_gate, exp_w_up, exp_w_down,
              rec_w_in, gla_alpha_up, gla_alpha_b, gla_norm_g, lru_conv_w, lru_conv_b, lru_w_a, lru_b_a, lru_w_i, lru_b_i,
              lru_lambda, na_w_qkv, na_rpb, norm_f_g):
    n_tok = x.shape[1]
    cos, sin = axial_rope(n_tok, GLA_DK)
    c_act = jax.nn.silu(c)
    cc_act = jax.nn.silu(c_ctx)
    for l in range(DEPTH):
        last = l == DEPTH - 1
        i = l // 2
        m_lat = jnp.split((c_act @ w_mod[l] + b_mod[l])[:, None, :], N_MOD, axis=-1)
        m_ctx = jnp.split((cc_act @ w_mod[l] + b_mod[l])[None, None, :], N_MOD, axis=-1)
        h_lat = modulate(rms_norm(x, norm_mix_g[l]), m_lat[0], m_lat[1])
        h_ctx = modulate(rms_norm(ctx, norm_mix_g[l]), m_ctx[0], m_ctx[1])
        if l % 2 == 0:
            y_ctx, y_lat = rec_mixer(h_ctx, h_lat, rec_w_in[i], gla_alpha_up[i], gla_alpha_b[i], gla_norm_g[i],
                                     lru_conv_w[i], lru_conv_b[i], lru_w_a[i], lru_b_a[i], lru_w_i[i], lru_b_i[i],
                                     lru_lambda[i], cos, sin, not last)
        else:
            y_ctx, y_lat = na_mixer(h_ctx, h_lat, na_w_qkv[i], na_rpb[i], not last)
        x = x + m_lat[2] * (y_lat @ w_out[l])
        x = x + m_lat[5] * expert_choice_ffn(modulate(rms_norm(x, norm_ffn_g[l]), m_lat[3], m_lat[4]),
                                             router_w[l], exp_w_gate[l], exp_w_up[l], exp_w_down[l])
        if not last:
            ctx = ctx + m_ctx[2] * (y_ctx @ w_out[l])
            ctx = ctx + m_ctx[5] * expert_choice_ffn(modulate(rms_norm(ctx, norm_ffn_g[l]), m_ctx[3], m_ctx[4]),
                                                     router_w[l], exp_w_gate[l], exp_w_up[l], exp_w_down[l])
    return rms_norm(x, norm_f_g)
```

```python
import contextlib
import numpy as np
import concourse.bass as bass
import concourse.mybir as mybir
from concourse.bass_utils import run_bass_kernel_spmd

F32 = mybir.dt.float32
BF16 = mybir.dt.bfloat16
U32 = mybir.dt.uint32
I32 = mybir.dt.int32
AF = mybir.ActivationFunctionType
ALU = mybir.AluOpType
AX = mybir.AxisListType

P = 128
D = 2048
NCH = D // P
TL = 2048
TC = 256
TS = TC + TL
NS = 2
NE = 16
FF = 1024
CAPL = 256
CAPC = 32
DEPTH = 4
EPS = 1e-6
NDS = 8
SELF_SYNC = True
NA_HEAD_BARRIER = False
NA_STAGES = 4
NA_PAIR = 2


class Buf:
    __slots__ = ("w", "r")

    def __init__(self):
        self.w = None
        self.r = {}


class Sched:
    def __init__(self, nc, es):
        self.nc = nc
        self.eng = {"pe": nc.tensor, "dve": nc.vector, "act": nc.scalar, "pool": nc.gpsimd, "sp": nc.sync}
        self.csem = {e: es.enter_context(nc.semaphore("c_" + e)) for e in ("pe", "dve", "act", "pool")}
        self.ccnt = {e: 0 for e in self.csem}
        self.dsem = {q: [es.enter_context(nc.semaphore("d_%s_%d" % (q, i))) for i in range(NDS)]
                     for q in ("sp", "pool", "act")}
        self.dcnt = {q: [0] * NDS for q in self.dsem}
        self.dnext = {q: 0 for q in self.dsem}
        self.waited = {}
        ss = SELF_SYNC
        self.self_sync = {"pe": False, "dve": ss, "act": ss, "pool": ss, "sp": True}

    def wait(self, e, ev):
        if ev is None:
            return
        sem, val, key = ev
        if key == ("c", e) and not self.self_sync[e]:
            return
        if self.waited.get((e, key), 0) >= val:
            return
        self.eng[e].wait_ge(sem, val)
        self.waited[(e, key)] = val

    def _deps(self, e, reads, writes):
        for b in reads:
            self.wait(e, b.w)
        for b in writes:
            self.wait(e, b.w)
            for ev in list(b.r.values()):
                self.wait(e, ev)

    def _mark(self, ev, reads, writes):
        for b in reads:
            old = b.r.get(ev[2])
            if old is None or old[1] < ev[1]:
                b.r[ev[2]] = ev
        for b in writes:
            b.w = ev
            b.r = {}

    def op(self, e, fn, reads=(), writes=()):
        self._deps(e, reads, writes)
        inst = fn(self.eng[e])
        self.ccnt[e] += 1
        inst.then_inc(self.csem[e], 1)
        ev = (self.csem[e], self.ccnt[e], ("c", e))
        self._mark(ev, reads, writes)
        return ev

    def dma(self, q, fn, reads=(), writes=()):
        self._deps(q, reads, writes)
        i = self.dnext[q]
        self.dnext[q] = (i + 1) % NDS
        sem = self.dsem[q][i]
        key = ("d", q, i)
        if self.dcnt[q][i] > 0:
            self.wait(q, (sem, self.dcnt[q][i], key))
        inst = fn(self.eng[q])
        self.dcnt[q][i] += 16
        inst.then_inc(sem, 16)
        ev = (sem, self.dcnt[q][i], key)
        self._mark(ev, reads, writes)
        return ev

    def barrier(self):
        evs = [(self.csem[o], self.ccnt[o], ("c", o)) for o in self.csem if self.ccnt[o] > 0]
        for q in self.dsem:
            for i in range(NDS):
                if self.dcnt[q][i] > 0:
                    evs.append((self.dsem[q][i], self.dcnt[q][i], ("d", q, i)))
        for e in ("pe", "dve", "act", "pool", "sp"):
            ss = self.self_sync[e]
            self.self_sync[e] = True
            for ev in evs:
                self.wait(e, ev)
            self.self_sync[e] = ss


class Ctx:
    pass


def group_tiles(tiles_stages, n):
    if n <= 1:
        return tiles_stages
    merged = []
    for p0 in range(0, len(tiles_stages), n):
        grp_ = tiles_stages[p0:p0 + n]

        def mk(si, grp_=grp_):
            def run():
                for ts_ in grp_:
                    if si < len(ts_):
                        ts_[si]()
            return run
        merged.append([mk(si) for si in range(max(len(g_) for g_ in grp_))])
    return merged


def run_pipelined(tiles_stages):
    n = len(tiles_stages)
    nst = max(len(s_) for s_ in tiles_stages)
    for step in range(n + nst - 1):
        for sidx in reversed(range(nst)):
            t = step - sidx
            if 0 <= t < n and sidx < len(tiles_stages[t]):
                tiles_stages[t][sidx]()


_UID = [0]


def uname(name):
    _UID[0] += 1
    return "%s_u%d" % (name, _UID[0])


def sb(nc, es, name, shape, dt):
    return es.enter_context(nc.sbuf_tensor(uname(name), shape, dt))


def norm_tile(k, S, xt, xb, hm, hb, small, smb, X, r0, grow, srow, gsb, rows=P):
    S.dma("sp", lambda q: q.dma_start(out=xt[:rows, :], in_=X[r0:r0 + rows, :]), writes=[xb])
    S.op("act", lambda a: a.activation(out=hm[:rows, :], in_=xt[:rows, :], func=AF.Square,
                                       accum_out=small[:rows, 0:1]),
         reads=[xb], writes=[hb, smb])
    S.op("dve", lambda v: v.tensor_scalar(small[:rows, 1:2], small[:rows, 0:1], 1.0 / D, EPS,
                                          op0=ALU.mult, op1=ALU.add), reads=[smb], writes=[smb])
    S.op("act", lambda a: a.activation(out=small[:rows, 2:3], in_=small[:rows, 1:2], func=AF.Sqrt),
         reads=[smb], writes=[smb])
    S.op("dve", lambda v: v.reciprocal(small[:rows, 3:4], small[:rows, 2:3]), reads=[smb], writes=[smb])
    S.op("dve", lambda v: v.scalar_tensor_tensor(out=hm[:rows, :], in0=xt[:rows, :], scalar=small[:rows, 3:4],
                                                 in1=grow[:rows, :], op0=ALU.mult, op1=ALU.mult),
         reads=[xb, smb, gsb], writes=[hb])
    S.op("pool", lambda g: g.tensor_tensor(out=hm[:rows, :], in0=hm[:rows, :], in1=srow[:rows, :], op=ALU.add),
         reads=[hb, gsb], writes=[hb])


def norm_stages(S, X, r0, xt, xb, hm, hb, small, smb, grow, srow, gsb):
    def sA():
        S.dma("sp", lambda q: q.dma_start(out=xt[:, :], in_=X[r0:r0 + P, :]), writes=[xb])
        S.op("act", lambda a: a.activation(out=hm[:, :], in_=xt[:, :], func=AF.Square, accum_out=small[:, 0:1]),
             reads=[xb], writes=[hb, smb])

    def sB():
        S.op("dve", lambda v: v.tensor_scalar(small[:, 1:2], small[:, 0:1], 1.0 / D, EPS, op0=ALU.mult, op1=ALU.add),
             reads=[smb], writes=[smb])
        S.op("act", lambda a: a.activation(out=small[:, 2:3], in_=small[:, 1:2], func=AF.Sqrt), reads=[smb], writes=[smb])
        S.op("dve", lambda v: v.reciprocal(small[:, 3:4], small[:, 2:3]), reads=[smb], writes=[smb])
        S.op("dve", lambda v: v.scalar_tensor_tensor(out=hm[:, :], in0=xt[:, :], scalar=small[:, 3:4], in1=grow[:, :],
                                                     op0=ALU.mult, op1=ALU.mult), reads=[xb, smb, gsb], writes=[hb])
        S.op("pool", lambda g: g.tensor_tensor(out=hm[:, :], in0=hm[:, :], in1=srow[:, :], op=ALU.add),
             reads=[hb, gsb], writes=[hb])
    return sA, sB


class PsumPool:
    def __init__(self, nc, es, n=8, dt=F32, width=512):
        self.tiles = [es.enter_context(nc.psum_tensor(uname("ps%d" % i), [P, width], dt)) for i in range(n)]
        self.bufs = [Buf() for _ in range(n)]
        self.i = 0
        self.n = n

    def get(self):
        i = self.i
        self.i = (i + 1) % self.n
        return self.tiles[i], self.bufs[i]


def ffn_phase(k, S, l, do_ctx=True):
    nc = k.nc
    X, H2, MODR = k.X, k.H2, k.MODR
    with contextlib.ExitStack() as es:
        PS = PsumPool(nc, es)
        ident = sb(nc, es, "f_ident", [P, P], F32)
        identb = Buf()
        S.dma("sp", lambda q: q.dma_start(out=ident[:], in_=k.ident_d[:, :]), writes=[identb])
        wr = sb(nc, es, "f_wr", [P, NCH, NE], F32)
        wrb = Buf()
        S.dma("sp", lambda q: q.dma_start(out=wr[:], in_=k.router_w[l].rearrange("(c p) e -> p c e", p=P)),
              writes=[wrb])
        idxl = sb(nc, es, "f_idxl", [P, 4, NE], I32)
        gatel = sb(nc, es, "f_gatel", [P, 4, NE], F32)
        idxc = sb(nc, es, "f_idxc", [P, NE], I32)
        gatec = sb(nc, es, "f_gatec", [P, NE], F32)
        tabb = Buf()
        with contextlib.ExitStack() as es2:
            NBN = 6
            xts = [sb(nc, es2, "fa_xt%d" % i, [P, D], F32) for i in range(NBN)]
            xbs = [Buf() for _ in range(NBN)]
            hms = [sb(nc, es2, "fa_hm%d" % i, [P, D], F32) for i in range(NBN)]
            hbs = [Buf() for _ in range(NBN)]
            smalls = [sb(nc, es2, "fa_sm%d" % i, [P, 8], F32) for i in range(NBN)]
            smbs = [Buf() for _ in range(NBN)]
            grow = sb(nc, es2, "fa_grow", [P, D], F32)
            srow = sb(nc, es2, "fa_srow", [P, D], F32)
            gnorm = sb(nc, es2, "fa_gnorm", [P, D], F32)
            gsb = Buf()
            gnb = Buf()
            hT = [sb(nc, es2, "fa_hT%d" % i, [P, NCH, P], F32) for i in range(NBN)]
            hTb = [Buf() for _ in range(NBN)]
            lg = [sb(nc, es2, "fa_lg%d" % i, [P, 64], F32) for i in range(NBN)]
            lgb = [Buf() for _ in range(NBN)]
            affl = sb(nc, es2, "fa_affl", [48, TL], F32)
            afflb = Buf()
            affc = sb(nc, es2, "fa_affc", [48, TC], F32)
            affcb = Buf()
            S.op("dve", lambda v: v.memset(affl[:], 0.0), writes=[afflb])
            S.op("dve", lambda v: v.memset(affc[:], 0.0), writes=[affcb])
            S.dma("sp", lambda q: q.dma_start(out=gnorm[:], in_=k.norm_ffn_g[l:l + 1, :].partition_broadcast(P)),
                  writes=[gnb])
            it = 0
            for s in range(NS):
                for grp in ((0, 1) if do_ctx else (1,)):
                    ms = 2 if grp == 0 else s
                    S.dma("sp", lambda q: q.dma_start(out=grow[:], in_=MODR[l, ms, 4:5, :].partition_broadcast(P)),
                          writes=[gsb])
                    S.dma("sp", lambda q: q.dma_start(out=srow[:], in_=MODR[l, ms, 3:4, :].partition_broadcast(P)),
                          writes=[gsb])
                    S.op("dve", lambda v: v.scalar_tensor_tensor(out=grow[:], in0=grow[:], scalar=1.0, in1=gnorm[:],
                                                                 op0=ALU.add, op1=ALU.mult),
                         reads=[gnb], writes=[gsb])
                    ntile = (TC if grp == 0 else TL) // P
                    rbase = s * TS + (0 if grp == 0 else TC)
                    tiles_st = []
                    for t in range(ntile):
                        j = it % NBN
                        it += 1
                        r0 = rbase + t * P
                        sA, sB = norm_stages(S, X, r0, xts[j], xbs[j], hms[j], hbs[j], smalls[j], smbs[j], grow, srow, gsb)
                        if grp == 0:
                            dst, dstb = affc[s * 32:s * 32 + NE, t * P:(t + 1) * P], affcb
                        else:
                            dst, dstb = affl[s * 32:s * 32 + NE, t * P:(t + 1) * P], afflb
                        hold = {}

                        def sC(j=j, r0=r0):
                            S.dma("sp", lambda q: q.dma_start(out=H2[r0:r0 + P, :], in_=hms[j][:, :]), reads=[hbs[j]])
                            for cb in range(4):
                                pt, pb = PS.get()
                                for c4 in range(4):
                                    c = cb * 4 + c4
                                    S.op("pe", lambda pe: pe.transpose(out=pt[:, c4 * P:(c4 + 1) * P],
                                                                       in_=hms[j][:, c * P:(c + 1) * P], identity=ident[:]),
                                         reads=[hbs[j], identb], writes=[pb])
                                src_ = pt[:, :].rearrange("p (c t) -> p c t", c=4)
                                if cb % 2 == 0:
                                    S.op("act", lambda a: a.activation(out=hT[j][:, cb * 4:cb * 4 + 4, :], in_=src_, func=AF.Copy),
                                         reads=[pb], writes=[hTb[j]])
                                else:
                                    S.op("dve", lambda v: v.tensor_copy(out=hT[j][:, cb * 4:cb * 4 + 4, :], in_=src_),
                                         reads=[pb], writes=[hTb[j]])

                        def sD(j=j, hold=hold):
                            pl, plb = PS.get()
                            for c in range(NCH):
                                S.op("pe", lambda pe: pe.matmul(pl[:, 0:NE], lhsT=hT[j][:, c, :], rhs=wr[:, c, :],
                                                                start=(c == 0), stop=(c == NCH - 1)),
                                     reads=[hTb[j], wrb], writes=[plb])
                            L = lg[j]
                            Lb = lgb[j]
                            S.op("dve", lambda v: v.tensor_reduce(out=L[:, 16:17], in_=pl[:, 0:NE], op=ALU.max, axis=AX.X),
                                 reads=[plb], writes=[Lb])
                            S.op("dve", lambda v: v.tensor_scalar(L[:, 17:18], L[:, 16:17], -1.0, None, op0=ALU.mult),
                                 reads=[Lb], writes=[Lb])
                            S.op("act", lambda a: a.activation(out=L[:, 0:NE], in_=pl[:, 0:NE], func=AF.Exp,
                                                               bias=L[:, 17:18], scale=1.0, accum_out=L[:, 18:19]),
                                 reads=[plb, Lb], writes=[Lb])
                            S.op("dve", lambda v: v.reciprocal(L[:, 19:20], L[:, 18:19]), reads=[Lb], writes=[Lb])
                            S.op("dve", lambda v: v.tensor_scalar(L[:, 32:32 + NE], L[:, 0:NE], L[:, 19:20], None,
                                                                  op0=ALU.mult), reads=[Lb], writes=[Lb])

                        def sE(j=j, dst=dst, dstb=dstb):
                            L = lg[j]
                            Lb = lgb[j]
                            pa, pab = PS.get()
                            S.op("pe", lambda pe: pe.transpose(out=pa[0:NE, 0:P], in_=L[:, 32:32 + NE], identity=ident[:]),
                                 reads=[Lb, identb], writes=[pab])
                            S.op("act", lambda a: a.activation(out=dst, in_=pa[0:NE, 0:P], func=AF.Copy),
                                 reads=[pab], writes=[dstb])
                        tiles_st.append([sA, sB, sC, sD, sE])
                    run_pipelined(group_tiles(tiles_st, 2))
            vals = sb(nc, es2, "fb_vals", [48, CAPL], F32)
            idxu = sb(nc, es2, "fb_idxu", [48, CAPL], U32)
            idxf = sb(nc, es2, "fb_idxf", [48, CAPL], F32)
            valsc = sb(nc, es2, "fb_valsc", [48, CAPC], F32)
            idxuc = sb(nc, es2, "fb_idxuc", [48, CAPC], U32)
            idxfc = sb(nc, es2, "fb_idxfc", [48, CAPC], F32)
            tkb = Buf()

            def topk(work, workb, vv, iu, cap):
                for r in range(cap // 8):
                    sl = slice(r * 8, r * 8 + 8)
                    S.op("dve", lambda v: v.max(out=vv[:, sl], in_=work), reads=[workb], writes=[tkb])
                    S.op("dve", lambda v: v.max_index(out=iu[:, sl], in_max=vv[:, sl], in_values=work),
                         reads=[workb, tkb], writes=[tkb])
                    S.op("dve", lambda v: v.match_replace(out=work, in_to_replace=vv[:, sl], in_values=work,
                                                          imm_value=-1.0), reads=[tkb], writes=[workb])

            topk(affl[:, :], afflb, vals, idxu, CAPL)
            S.op("dve", lambda v: v.tensor_copy(out=idxf[:, :], in_=idxu[:, :]), reads=[tkb], writes=[tkb])
            for s in range(NS):
                S.op("dve", lambda v: v.tensor_scalar(idxf[s * 32:s * 32 + NE, :], idxf[s * 32:s * 32 + NE, :],
                                                      float(s * TS + TC), None, op0=ALU.add), reads=[tkb], writes=[tkb])
            if do_ctx:
                topk(affc[:, :], affcb, valsc, idxuc, CAPC)
                S.op("dve", lambda v: v.tensor_copy(out=idxfc[:, :], in_=idxuc[:, :]), reads=[tkb], writes=[tkb])
                S.op("dve", lambda v: v.tensor_scalar(idxfc[32:32 + NE, :], idxfc[32:32 + NE, :],
                                                      float(TS), None, op0=ALU.add), reads=[tkb], writes=[tkb])
            for c in range(2):
                for (src_, dst) in ((idxf, idxl), (vals, gatel)):
                    pt, pb = PS.get()
                    S.op("pe", lambda pe: pe.transpose(out=pt[:, 0:48], in_=src_[:, c * P:(c + 1) * P],
                                                       identity=ident[0:48, 0:48]),
                         reads=[tkb, identb], writes=[pb])
                    for s in range(NS):
                        S.op("dve", lambda v: v.tensor_copy(out=dst[:, s * 2 + c, :], in_=pt[:, s * 32:s * 32 + NE]), reads=[pb], writes=[tabb])
            if do_ctx:
                for (src_, dst) in ((idxfc, idxc), (valsc, gatec)):
                    pt, pb = PS.get()
                    S.op("pe", lambda pe: pe.transpose(out=pt[0:CAPC, 0:48], in_=src_[:, :], identity=ident[0:48, 0:48]),
                         reads=[tkb, identb], writes=[pb])
                    S.op("dve", lambda v: v.tensor_copy(out=dst[0:CAPC, :], in_=pt[0:CAPC, 0:NE]), reads=[pb], writes=[tabb])
                    S.op("dve", lambda v: v.tensor_copy(out=dst[CAPC:2 * CAPC, :], in_=pt[0:CAPC, 32:32 + NE]), reads=[pb], writes=[tabb])
        S.barrier()
        with contextlib.ExitStack() as es3:
            NWB = 6
            wbuf = [sb(nc, es3, "fc_w%d" % i, [P, 16 * 512], BF16) for i in range(NWB)]
            wbb = [Buf() for _ in range(NWB)]
            wi = [0]
            xg = [sb(nc, es3, "fc_xg%d" % i, [P, D], F32) for i in range(2)]
            xgb = [Buf(), Buf()]
            xsT = [sb(nc, es3, "fc_xsT%d" % i, [P, NCH, 576], BF16) for i in range(2)]
            xsTb = [Buf(), Buf()]
            hidT = sb(nc, es3, "fc_hidT", [P, 8, 576], BF16)
            hidTb = Buf()
            sg = [sb(nc, es3, "fc_sg%d" % i, [P, 576], F32) for i in range(2)]
            sgb = [Buf(), Buf()]
            ysb = [sb(nc, es3, "fc_y%d" % i, [P, D], F32) for i in range(2)]
            ysbb = [Buf(), Buf()]
            m5 = [sb(nc, es3, "fc_m5_%d" % i, [P, D], F32) for i in range(3)]
            m5b = Buf()
            for ms in range(3):
                S.dma("sp", lambda q: q.dma_start(out=m5[ms][:], in_=MODR[l, ms, 5:6, :].partition_broadcast(P)),
                      writes=[m5b])
            xrow = [Buf(), Buf(), Buf()]
            ncols = 576 if do_ctx else 512
            halves = ((0, 512), (512, 576)) if do_ctx else ((0, 512),)

            def wload(src_ap, shape3):
                i = wi[0] % NWB
                wi[0] += 1
                a, b = shape3
                view = wbuf[i][:, 0:a * b].rearrange("p (a b) -> p a b", a=a)
                S.dma("pool", lambda q: q.dma_start(out=view, in_=src_ap), writes=[wbb[i]])
                return view, wbb[i]

            tiles = [(rt, P, rt * P) for rt in range(4)] + ([(4, NS * CAPC, 512)] if do_ctx else [])
            cnt = {"gi": 0, "yi": 0}

            def stage_gather(e):
                xs_, xsb_ = xsT[e % 2], xsTb[e % 2]
                for (rt, rows, c0) in tiles:
                    j = cnt["gi"] % 2
                    cnt["gi"] += 1
                    idx_ap = idxl[:, rt, e:e + 1] if rt < 4 else idxc[0:rows, e:e + 1]
                    S.dma("pool", lambda q: q.indirect_dma_start(
                        out=xg[j][0:rows, :], out_offset=None, in_=H2[:, :],
                        in_offset=bass.IndirectOffsetOnAxis(ap=idx_ap, axis=0)),
                        reads=[tabb], writes=[xgb[j]])
                    for cb in range(4):
                        pt, pb = PS.get()
                        for c4 in range(4):
                            c = cb * 4 + c4
                            S.op("pe", lambda pe: pe.transpose(out=pt[:, c4 * P:c4 * P + rows],
                                                               in_=xg[j][0:rows, c * P:(c + 1) * P],
                                                               identity=ident[0:rows, 0:rows]),
                                 reads=[xgb[j], identb], writes=[pb])
                        src_ = pt[:, :].rearrange("p (c t) -> p c t", c=4)[:, :, 0:rows]
                        dst = xs_[:, cb * 4:cb * 4 + 4, c0:c0 + rows]
                        if cb % 2 == 0:
                            S.op("act", lambda a: a.activation(out=dst, in_=src_, func=AF.Copy), reads=[pb], writes=[xsb_])
                        else:
                            S.op("dve", lambda v: v.tensor_copy(out=dst, in_=src_), reads=[pb], writes=[xsb_])

            def stage_gateup(e):
                xs_, xsb_ = xsT[e % 2], xsTb[e % 2]
                for fb in range(2):
                    wg, wgb = wload(k.exp_w_gate[l, e, :, fb * 512:(fb + 1) * 512].rearrange("(c p) f -> p c f", p=P), (16, 512))
                    wu, wub = wload(k.exp_w_up[l, e, :, fb * 512:(fb + 1) * 512].rearrange("(c p) f -> p c f", p=P), (16, 512))
                    for f4 in range(4):
                        fc = fb * 4 + f4
                        pgs = [PS.get() for _ in halves]
                        pus = [PS.get() for _ in halves]
                        for (w_, wb_, pp) in ((wg, wgb, pgs), (wu, wub, pus)):
                            for c in range(NCH):
                                for hi, (h0, h1) in enumerate(halves):
                                    S.op("pe", lambda pe: pe.matmul(pp[hi][0][:, 0:h1 - h0], lhsT=w_[:, c, f4 * P:(f4 + 1) * P],
                                                                    rhs=xs_[:, c, h0:h1], start=(c == 0), stop=(c == NCH - 1)),
                                         reads=[wb_, xsb_], writes=[pp[hi][1]])
                        for hi, (h0, h1) in enumerate(halves):
                            pg, pgb = pgs[hi]
                            pu, pub = pus[hi]
                            j = (fc + (h0 > 0)) % 2
                            S.op("act", lambda a: a.activation(out=sg[j][:, 0:h1 - h0], in_=pg[:, 0:h1 - h0], func=AF.Silu),
                                 reads=[pgb], writes=[sgb[j]])
                            S.op("dve", lambda v: v.tensor_tensor(out=hidT[:, fc, h0:h1], in0=sg[j][:, 0:h1 - h0],
                                                                  in1=pu[:, 0:h1 - h0], op=ALU.mult),
                                 reads=[sgb[j], pub], writes=[hidTb])

            def stage_down(e):
                wds = []
                for hb in range(2):
                    wds.append(wload(k.exp_w_down[l, e, hb * 512:(hb + 1) * 512, :].rearrange("(c p) d -> p c d", p=P), (4, D)))
                for (rt, rows, c0) in tiles:
                    j = cnt["yi"] % 2
                    cnt["yi"] += 1
                    gate_ap = gatel[:, rt, e:e + 1] if rt < 4 else gatec[0:rows, e:e + 1]
                    mrow = m5[rt // 2] if rt < 4 else m5[2]
                    pys = [PS.get() for _ in range(4)]
                    for fc in range(8):
                        wd, wdb = wds[fc // 4]
                        for cb in range(4):
                            S.op("pe", lambda pe: pe.matmul(pys[cb][0][0:rows, :], lhsT=hidT[:, fc, c0:c0 + rows],
                                                            rhs=wd[:, fc % 4, cb * 512:(cb + 1) * 512],
                                                            start=(fc == 0), stop=(fc == 7)),
                                 reads=[hidTb, wdb], writes=[pys[cb][1]])
                    for cb in range(4):
                        py, pyb = pys[cb]
                        S.op("dve", lambda v: v.scalar_tensor_tensor(out=ysb[j][0:rows, cb * 512:(cb + 1) * 512],
                                                                     in0=py[0:rows, :], scalar=gate_ap,
                                                                     in1=mrow[0:rows, cb * 512:(cb + 1) * 512],
                                                                     op0=ALU.mult, op1=ALU.mult),
                             reads=[pyb, tabb, m5b], writes=[ysbb[j]])
                    idx_ap = idxl[:, rt, e:e + 1] if rt < 4 else idxc[0:rows, e:e + 1]
                    xb_ = xrow[rt // 2] if rt < 4 else xrow[2]
                    S.dma("pool", lambda q: q.indirect_dma_start(
                        out=X[:, :], out_offset=bass.IndirectOffsetOnAxis(ap=idx_ap, axis=0),
                        in_=ysb[j][0:rows, :], in_offset=None, compute_op=ALU.add),
                        reads=[tabb, ysbb[j]], writes=[xb_])

            for e in range(NE):
                stage_gather(e)
                stage_gateup(e)
                stage_down(e)
        S.barrier()


def mod_phase(k, S, layers=range(DEPTH)):
    nc = k.nc
    with contextlib.ExitStack() as es:
        PS = PsumPool(nc, es)
        ident = sb(nc, es, "m_ident", [P, P], F32)
        identb = Buf()
        S.dma("sp", lambda q: q.dma_start(out=ident[:], in_=k.ident_d[:, :]), writes=[identb])
        craw = sb(nc, es, "m_craw", [4, D], F32)
        cact = sb(nc, es, "m_cact", [4, D], F32)
        cb_ = Buf()
        S.op("dve", lambda v: v.memset(craw[:], 0.0), writes=[cb_])
        S.dma("sp", lambda q: q.dma_start(out=craw[0:2, :], in_=k.c[:, :]), writes=[cb_])
        S.dma("sp", lambda q: q.dma_start(out=craw[2:3, :], in_=k.c_ctx[:, :]), writes=[cb_])
        S.op("act", lambda a: a.activation(out=cact[:], in_=craw[:], func=AF.Silu), reads=[cb_], writes=[cb_])
        cT = sb(nc, es, "m_cT", [P, NCH, 4], BF16)
        cTb = Buf()
        for c in range(NCH):
            pt, pb = PS.get()
            S.op("pe", lambda pe: pe.transpose(out=pt[:, 0:4], in_=cact[:, c * P:(c + 1) * P], identity=ident[0:4, 0:4]),
                 reads=[cb_, identb], writes=[pb])
            S.op("dve", lambda v: v.tensor_copy(out=cT[:, c, :], in_=pt[:, 0:4]), reads=[pb], writes=[cTb])
        wts = [sb(nc, es, "m_w%d" % i, [P, NCH, 512], BF16) for i in range(4)]
        wtb = [Buf() for _ in range(4)]
        bm = [sb(nc, es, "m_b%d" % i, [4, 512], F32) for i in range(2)]
        bmb = [Buf(), Buf()]
        ot = [sb(nc, es, "m_o%d" % i, [4, 512], F32) for i in range(2)]
        otb = [Buf(), Buf()]
        it = 0
        for l in layers:
            for cb in range(24):
                j = it % 4
                j2 = it % 2
                it += 1
                v_, c0 = cb // 4, (cb % 4) * 512
                S.dma("pool", lambda q: q.dma_start(
                    out=wts[j][:, :, :],
                    in_=k.w_mod[l, :, cb * 512:(cb + 1) * 512].rearrange("(c p) n -> p c n", p=P)),
                    writes=[wtb[j]])
                S.dma("sp", lambda q: q.dma_start(out=bm[j2][:], in_=k.b_mod[l:l + 1, cb * 512:(cb + 1) * 512].partition_broadcast(4)),
                      writes=[bmb[j2]])
                pm, pmb = PS.get()
                for c in range(NCH):
                    S.op("pe", lambda pe: pe.matmul(pm[0:4, :], lhsT=cT[:, c, :], rhs=wts[j][:, c, :],
                                                    start=(c == 0), stop=(c == NCH - 1)),
                         reads=[cTb, wtb[j]], writes=[pmb])
                S.op("dve", lambda v: v.tensor_tensor(out=ot[j2][:], in0=pm[0:4, :], in1=bm[j2][:], op=ALU.add),
                     reads=[pmb, bmb[j2]], writes=[otb[j2]])
                S.dma("sp", lambda q: q.dma_start(out=k.MODR[l, 0:3, v_, c0:c0 + 512], in_=ot[j2][0:3, :]), reads=[otb[j2]])
    S.barrier()


def norm1_phase(k, S, l, s, HT, HTb, PS, ident, identb, do_ctx=True):
    nc = k.nc
    NBN = 6
    with contextlib.ExitStack() as es2:
        xts = [sb(nc, es2, "n1_xt%d" % i, [P, D], F32) for i in range(NBN)]
        xbs = [Buf() for _ in range(NBN)]
        hms = [sb(nc, es2, "n1_hm%d" % i, [P, D], F32) for i in range(NBN)]
        hbs = [Buf() for _ in range(NBN)]
        smalls = [sb(nc, es2, "n1_sm%d" % i, [P, 8], F32) for i in range(NBN)]
        smbs = [Buf() for _ in range(NBN)]
        grow = sb(nc, es2, "n1_grow", [P, D], F32)
        srow = sb(nc, es2, "n1_srow", [P, D], F32)
        gnorm = sb(nc, es2, "n1_gnorm", [P, D], F32)
        gsb = Buf()
        gnb = Buf()
        S.dma("sp", lambda q: q.dma_start(out=gnorm[:], in_=k.norm_mix_g[l:l + 1, :].partition_broadcast(P)), writes=[gnb])
        it = 0
        for grp in ((0, 1) if do_ctx else (1,)):
            ms = 2 if grp == 0 else s
            S.dma("sp", lambda q: q.dma_start(out=grow[:], in_=k.MODR[l, ms, 1:2, :].partition_broadcast(P)), writes=[gsb])
            S.dma("sp", lambda q: q.dma_start(out=srow[:], in_=k.MODR[l, ms, 0:1, :].partition_broadcast(P)), writes=[gsb])
            S.op("dve", lambda v: v.scalar_tensor_tensor(out=grow[:], in0=grow[:], scalar=1.0, in1=gnorm[:],
                                                         op0=ALU.add, op1=ALU.mult), reads=[gnb], writes=[gsb])
            ntile = (TC if grp == 0 else TL) // P
            rbase = s * TS + (0 if grp == 0 else TC)
            tiles_st = []
            for t in range(ntile):
                j = it % NBN
                it += 1
                r0 = rbase + t * P
                col0 = (0 if grp == 0 else TC) + t * P
                sA, sB = norm_stages(S, k.X, r0, xts[j], xbs[j], hms[j], hbs[j], smalls[j], smbs[j], grow, srow, gsb)

                def sC(j=j, col0=col0):
                    for cb in range(4):
                        pt, pb = PS.get()
                        for c4 in range(4):
                            c = cb * 4 + c4
                            S.op("pe", lambda pe: pe.transpose(out=pt[:, c4 * P:(c4 + 1) * P], in_=hms[j][:, c * P:(c + 1) * P],
                                                               identity=ident[:]), reads=[hbs[j], identb], writes=[pb])
                        src_ = pt[:, :].rearrange("p (c t) -> p c t", c=4)
                        dst = HT[:, cb * 4:cb * 4 + 4, col0:col0 + P]
                        if cb % 2 == 0:
                            S.op("act", lambda a: a.activation(out=dst, in_=src_, func=AF.Copy), reads=[pb], writes=[HTb])
                        else:
                            S.op("dve", lambda v: v.tensor_copy(out=dst, in_=src_), reads=[pb], writes=[HTb])
                tiles_st.append([sA, sB, sC])
            run_pipelined(group_tiles(tiles_st, 2))


def outproj_phase(k, S, l, s, do_ctx=True):
    nc = k.nc
    with contextlib.ExitStack() as es:
        PS = PsumPool(nc, es)
        YT = sb(nc, es, "op_YT", [P, NCH, TS], BF16)
        YTb = Buf()
        for c in range(NCH):
            S.dma("sp", lambda q: q.dma_start(out=YT[:, c, :], in_=k.YTd[c, :, :]), writes=[YTb])
        m2 = [sb(nc, es, "op_m2_%d" % i, [P, D], F32) for i in range(2)]
        m2b = Buf()
        S.dma("sp", lambda q: q.dma_start(out=m2[0][:], in_=k.MODR[l, 2, 2:3, :].partition_broadcast(P)), writes=[m2b])
        S.dma("sp", lambda q: q.dma_start(out=m2[1][:], in_=k.MODR[l, s, 2:3, :].partition_broadcast(P)), writes=[m2b])
        wts = [sb(nc, es, "op_w%d" % i, [P, NCH, 512], BF16) for i in range(2)]
        wtb = [Buf(), Buf()]
        xt = [sb(nc, es, "op_x%d" % i, [P, 512], F32) for i in range(3)]
        xtb = [Buf(), Buf(), Buf()]
        it = 0
        for cb in range(4):
            j = cb % 2
            S.dma("pool", lambda q: q.dma_start(out=wts[j][:], in_=k.w_out[l, :, cb * 512:(cb + 1) * 512].rearrange("(c p) n -> p c n", p=P)),
                  writes=[wtb[j]])
            for t in range(0 if do_ctx else 2, TS // P):
                i = it % 3
                it += 1
                r0 = s * TS + t * P
                S.dma("sp", lambda q: q.dma_start(out=xt[i][:], in_=k.X[r0:r0 + P, cb * 512:(cb + 1) * 512]), writes=[xtb[i]])
                py, pyb = PS.get()
                for c in range(NCH):
                    S.op("pe", lambda pe: pe.matmul(py[:, :], lhsT=YT[:, c, t * P:(t + 1) * P], rhs=wts[j][:, c, :],
                                                    start=(c == 0), stop=(c == NCH - 1)), reads=[YTb, wtb[j]], writes=[pyb])
                mrow = m2[0] if t < 2 else m2[1]
                tmp = py
                S.op("dve", lambda v: v.tensor_tensor(out=py[:, :], in0=py[:, :], in1=mrow[:, cb * 512:(cb + 1) * 512], op=ALU.mult),
                     reads=[m2b], writes=[pyb])
                S.op("dve", lambda v: v.tensor_tensor(out=xt[i][:], in0=py[:, :], in1=xt[i][:], op=ALU.add),
                     reads=[pyb], writes=[xtb[i]])
                S.dma("sp", lambda q: q.dma_start(out=k.X[r0:r0 + P, cb * 512:(cb + 1) * 512], in_=xt[i][:]), reads=[xtb[i]])
    S.barrier()


def na_band_start(i):
    return min(max(2 * i - 4, 0), 22)


def na_tab_type(i):
    return {0: 0, 1: 1, 14: 3, 15: 4}.get(i, 2)


def na_phase(k, S, l, s, do_ctx=True):
    nc = k.nc
    ia = l // 2
    scale = 128 ** -0.5
    NB = 4
    with contextlib.ExitStack() as es:
        PS = PsumPool(nc, es, n=6)
        PSB = PsumPool(nc, es, n=2, dt=BF16, width=1024)
        ident = sb(nc, es, "na_ident", [P, P], F32)
        identb = Buf()
        S.dma("sp", lambda q: q.dma_start(out=ident[:], in_=k.ident_d[:, :]), writes=[identb])
        ident16 = sb(nc, es, "na_ident16", [P, P], BF16)
        S.op("dve", lambda v: v.tensor_copy(out=ident16[:], in_=ident[:]), reads=[identb], writes=[identb])
        HT = sb(nc, es, "na_HT", [P, NCH, TS], BF16)
        HTb = Buf()
        norm1_phase(k, S, l, s, HT, HTb, PS, ident, identb, do_ctx=True)
        S.barrier()
        wq = [sb(nc, es, "na_wq%d" % i, [P, NCH, 2, P], BF16) for i in range(2)]
        wqb = [Buf(), Buf()]
        tab = [sb(nc, es, "na_tab%d" % i, [P, 5, 640], F32) for i in range(2)]
        tabb = [Buf(), Buf()]
        QT = sb(nc, es, "na_QT", [P, TS], BF16)
        KT = sb(nc, es, "na_KT", [P, TS], BF16)
        V4 = sb(nc, es, "na_V4", [P, TS // P, 4 * P], BF16)
        V4b = Buf()
        wv4 = sb(nc, es, "na_wv4", [P, NCH, 4 * P], BF16)
        wv4b = Buf()
        qkvb = Buf()
        YTh = [sb(nc, es, "na_YTh%d" % i, [P, TS], BF16) for i in range(2)]
        YThb = [Buf(), Buf()]
        NB1, NB2, NB3 = 6, 6, 5
        sc = [sb(nc, es, "na_sc%d" % i, [P, 896], F32) for i in range(NB1)]
        scb = [[Buf(), Buf(), Buf()] for _ in range(NB1)]
        pbf = [sb(nc, es, "na_pbf%d" % i, [P, 896], BF16) for i in range(NB2)]
        pbfb = [Buf() for _ in range(NB2)]
        st = [sb(nc, es, "na_st%d" % i, [P, 8], F32) for i in range(NB1)]
        stb = [Buf() for _ in range(NB1)]
        PT = [sb(nc, es, "na_PT%d" % i, [P, 7, P], BF16) for i in range(NB3)]
        PTb = [Buf() for _ in range(NB3)]
        if not do_ctx:
            for i in range(2):
                S.op("dve", lambda v: v.memset(YTh[i][:, 0:TC], 0.0), writes=[YThb[i]])
        it = 0
        for h in range(16):
            j = h % 2
            if h % 4 == 0:
                S.dma("pool", lambda q: q.dma_start(
                    out=wv4[:, :, :],
                    in_=k.na_w_qkv[ia, :, 2 * D + h * P:2 * D + (h + 4) * P].rearrange("(c p) n -> p c n", p=P)), writes=[wv4b])
                for t in range(TS // P):
                    pv, pvb = PS.get()
                    for c in range(NCH):
                        S.op("pe", lambda pe: pe.matmul(pv[:, :], lhsT=HT[:, c, t * P:(t + 1) * P], rhs=wv4[:, c, :],
                                                        start=(c == 0), stop=(c == NCH - 1)), reads=[wv4b, HTb], writes=[pvb])
                    if t % 2 == 0:
                        S.op("act", lambda a: a.activation(out=V4[:, t, :], in_=pv[:, :], func=AF.Copy), reads=[pvb], writes=[V4b])
                    else:
                        S.op("dve", lambda v: v.tensor_copy(out=V4[:, t, :], in_=pv[:, :]), reads=[pvb], writes=[V4b])
            for qi in range(2):
                S.dma("pool", lambda q: q.dma_start(
                    out=wq[j][:, :, qi, :],
                    in_=k.na_w_qkv[ia, :, qi * D + h * P:qi * D + (h + 1) * P].rearrange("(c p) n -> p c n", p=P)),
                    writes=[wqb[j]])
            S.dma("sp", lambda q: q.dma_start(out=tab[j][:], in_=k.na_tab[ia, h].rearrange("t p n -> p t n")), writes=[tabb[j]])
            for qi, dstT in ((0, QT), (1, KT)):
                for tb in range(5):
                    n0 = tb * 512
                    n1 = min(TS, n0 + 512)
                    pq, pqb = PS.get()
                    for c in range(NCH):
                        S.op("pe", lambda pe: pe.matmul(pq[:, 0:n1 - n0], lhsT=wq[j][:, c, qi, :], rhs=HT[:, c, n0:n1],
                                                        start=(c == 0), stop=(c == NCH - 1)), reads=[wqb[j], HTb], writes=[pqb])
                    sc_ = scale if qi == 0 else 1.0
                    if tb % 2 == 0:
                        S.op("act", lambda a: a.activation(out=dstT[:, n0:n1], in_=pq[:, 0:n1 - n0], func=AF.Copy, scale=sc_),
                             reads=[pqb], writes=[qkvb])
                    else:
                        S.op("dve", lambda v: v.tensor_scalar(dstT[:, n0:n1], pq[:, 0:n1 - n0], sc_, None, op0=ALU.mult),
                             reads=[pqb], writes=[qkvb])
            qtiles = ([("c", 0), ("c", 1)] if do_ctx else []) + [("l", i) for i in range(16)]

            def make_tile(kind, i, tix, j=j, hq=h % 4):
                u1, u2, u3 = tix % NB1, tix % NB2, tix % NB3
                par = tix % 2
                hold = {}
                if kind == "c":
                    q0, nk, vtiles = i * P, 256, [0, 1]
                else:
                    q0, nk = TC + i * P, 896
                    bs = na_band_start(i)
                    k0 = TC + bs * 64
                    tt = na_tab_type(i)
                    vtiles = [2 + bs // 2 + c for c in range(5)] + [0, 1]
                nchunk = nk // P

                def s1():
                    pa, pab = PS.get()
                    hold["pa"] = (pa, pab)
                    if kind == "c":
                        S.op("pe", lambda pe: pe.matmul(pa[:, 0:256], lhsT=QT[:, q0:q0 + P], rhs=KT[:, 0:256], start=True, stop=True),
                             reads=[qkvb], writes=[pab])
                    else:
                        pb2, pb2b = PS.get()
                        hold["pb2"] = (pb2, pb2b)
                        S.op("pe", lambda pe: pe.matmul(pa[:, 0:512], lhsT=QT[:, q0:q0 + P], rhs=KT[:, k0:k0 + 512], start=True, stop=True),
                             reads=[qkvb], writes=[pab])
                        S.op("pe", lambda pe: pe.matmul(pb2[:, 0:128], lhsT=QT[:, q0:q0 + P], rhs=KT[:, k0 + 512:k0 + 640], start=True, stop=True),
                             reads=[qkvb], writes=[pb2b])
                        S.op("pe", lambda pe: pe.matmul(pb2[:, 128:384], lhsT=QT[:, q0:q0 + P], rhs=KT[:, 0:256], start=True, stop=True),
                             reads=[qkvb], writes=[pb2b])

                def s2():
                    pa, pab = hold["pa"]
                    if kind == "c":
                        S.op("act", lambda a: a.activation(out=sc[u1][:, 0:256], in_=pa[:, 0:256], func=AF.Copy),
                             reads=[pab], writes=[scb[u1][0]])
                    else:
                        pb2, pb2b = hold["pb2"]
                        S.op("dve", lambda v: v.tensor_tensor(out=sc[u1][:, 0:512], in0=pa[:, 0:512], in1=tab[j][:, tt, 0:512], op=ALU.add),
                             reads=[pab, tabb[j]], writes=[scb[u1][0]])
                        S.op("dve", lambda v: v.tensor_tensor(out=sc[u1][:, 512:640], in0=pb2[:, 0:128], in1=tab[j][:, tt, 512:640], op=ALU.add),
                             reads=[pb2b, tabb[j]], writes=[scb[u1][1]])
                        S.op("act", lambda a: a.activation(out=sc[u1][:, 640:896], in_=pb2[:, 128:384], func=AF.Copy),
                             reads=[pb2b], writes=[scb[u1][2]])

                def s3():
                    S.op("dve", lambda v: v.tensor_reduce(out=st[u1][:, 1:2], in_=sc[u1][:, 0:nk], op=ALU.max, axis=AX.X),
                         reads=scb[u1], writes=[stb[u1]])
                    S.op("dve", lambda v: v.tensor_scalar(st[u1][:, 0:1], st[u1][:, 1:2], -1.0, None, op0=ALU.mult),
                         reads=[stb[u1]], writes=[stb[u1]])

                def s4():
                    S.op("act", lambda a: a.activation(out=sc[u1][:, 0:nk], in_=sc[u1][:, 0:nk], func=AF.Exp, bias=st[u1][:, 0:1], scale=1.0,
                                                       accum_out=st[u1][:, 2:3]), reads=[stb[u1]], writes=scb[u1] + [stb[u1]])

                def s5():
                    S.op("dve", lambda v: v.reciprocal(st[u1][:, 3:4], st[u1][:, 2:3]), reads=[stb[u1]], writes=[stb[u1]])
                    if par == 0:
                        S.op("act", lambda a: a.activation(out=pbf[u2][:, 0:nk], in_=sc[u1][:, 0:nk], func=AF.Copy, scale=st[u1][:, 3:4]),
                             reads=[stb[u1]] + scb[u1], writes=[pbfb[u2]])
                    else:
                        S.op("dve", lambda v: v.tensor_scalar(pbf[u2][:, 0:nk], sc[u1][:, 0:nk], st[u1][:, 3:4], None, op0=ALU.mult),
                             reads=[stb[u1]] + scb[u1], writes=[pbfb[u2]])

                def s6():
                    pt, ptb = PSB.get()
                    hold["pt"] = (pt, ptb)
                    for c in range(nchunk):
                        S.op("pe", lambda pe: pe.transpose(out=pt[:, c * P:(c + 1) * P], in_=pbf[u2][:, c * P:(c + 1) * P], identity=ident16[:]),
                             reads=[pbfb[u2], identb], writes=[ptb])

                def s7():
                    pt, ptb = hold["pt"]
                    S.op("act", lambda a: a.activation(out=PT[u3][:, 0:nchunk, :], in_=pt[:, 0:nk].rearrange("p (c t) -> p c t", c=nchunk), func=AF.Copy),
                         reads=[ptb], writes=[PTb[u3]])

                def s8():
                    po, pob = PS.get()
                    hold["po"] = (po, pob)
                    for c in range(nchunk):
                        S.op("pe", lambda pe: pe.matmul(po[:, 0:P], lhsT=V4[:, vtiles[c], hq * P:(hq + 1) * P], rhs=PT[u3][:, c, :],
                                                        start=(c == 0), stop=(c == nchunk - 1)), reads=[V4b, PTb[u3]], writes=[pob])

                def s9():
                    po, pob = hold["po"]
                    S.op("dve", lambda v: v.tensor_copy(out=YTh[j][:, q0:q0 + P], in_=po[:, 0:P]), reads=[pob], writes=[YThb[j]])
                def grp(*fs):
                    def run():
                        for f_ in fs:
                            f_()
                    return run
                if NA_STAGES == 4:
                    return [grp(s1, s2, s3), grp(s4, s5), grp(s6, s7), grp(s8, s9)]
                return [s1, s2, s3, s4, s5, s6, s7, s8, s9]

            tiles_st = []
            for (kind, i) in qtiles:
                tiles_st.append(make_tile(kind, i, it))
                it += 1
            if NA_PAIR > 1:
                merged = []
                for p0 in range(0, len(tiles_st), NA_PAIR):
                    grp_ = tiles_st[p0:p0 + NA_PAIR]

                    def mk(si, grp_=grp_):
                        def run():
                            for ts_ in grp_:
                                ts_[si]()
                        return run
                    merged.append([mk(si) for si in range(len(grp_[0]))])
                tiles_st = merged
            run_pipelined(tiles_st)
            if NA_HEAD_BARRIER:
                S.barrier()
            S.dma("sp", lambda q: q.dma_start(out=k.YTd[h, :, :], in_=YTh[j][:, :]), reads=[YThb[j]])
    S.barrier()


def build_na_tab(rpb):
    A = rpb.shape[0]
    tab = np.full((A, 16, 5, 128, 640), -30000.0, np.float32)
    for tt, i in enumerate([0, 1, 2, 14, 15]):
        bs = na_band_start(i)
        for rr in range(2):
            r = 2 * i + rr
            rs = min(max(r - 4, 0), 24)
            for c in range(64):
                cs = min(max(c - 8, 0), 48)
                q = rr * 64 + c
                kc = np.arange(cs, cs + 16)
                for j in range(10):
                    kr = bs + j
                    if not (rs <= kr < rs + 8):
                        continue
                    tab[:, :, tt, q, j * 64 + kc] = rpb[:, :, kr - r + 7, kc - c + 15]
    return tab


REC_BLOCKS = [(0, 256), (256, 768), (768, 1280), (1280, 1792), (1792, 2304)]
C_Q, C_K, C_V, C_G, C_ZA, C_LX, C_LG = 0, 512, 1024, 2048, 3072, 3104, 4128


def build_rope_tabs():
    t = np.arange(TL)
    rows = (t // 64).astype(np.float32)
    cols = (t % 64).astype(np.float32)
    inv = (np.float32(10000.0) ** (-np.arange(32, dtype=np.float32) / np.float32(32))).astype(np.float32)
    ang = np.concatenate([rows[:, None] * inv, cols[:, None] * inv], axis=-1).astype(np.float32)
    cosT = np.cos(ang).astype(np.float32).T
    sinT = np.sin(ang).astype(np.float32).T
    cosF = np.repeat(cosT, 2, axis=0)
    sinF = np.repeat(sinT, 2, axis=0).copy()
    sinF[0::2] *= -1.0
    return np.ascontiguousarray(cosF), np.ascontiguousarray(sinF)


def proj_fm(S, PS, HT, HTb, wt, wtb, wsl, evac, blocks=REC_BLOCKS, m=P):
    for bi, (n0, n1) in enumerate(blocks):
        pq, pqb = PS.get()
        for c in range(NCH):
            S.op("pe", lambda pe: pe.matmul(pq[0:m, 0:n1 - n0], lhsT=wsl(c), rhs=HT[:, c, n0:n1],
                                            start=(c == 0), stop=(c == NCH - 1)), reads=[wtb, HTb], writes=[pqb])
        evac(pq, pqb, n0, n1, bi)


def rec_phase(k, S, l, s):
    nc = k.nc
    ir = l // 2
    NCK = TS // P
    with contextlib.ExitStack() as es:
        PS = PsumPool(nc, es, n=5)
        PSK = PsumPool(nc, es, n=3)
        ident = sb(nc, es, "r_ident", [P, P], F32)
        identb = Buf()
        S.dma("sp", lambda q: q.dma_start(out=ident[:], in_=k.ident_d[:, :]), writes=[identb])
        HT = sb(nc, es, "r_HT", [P, NCH, TS], BF16)
        HTb = Buf()
        norm1_phase(k, S, l, s, HT, HTb, PS, ident, identb, do_ctx=True)
        S.barrier()
        cst = Buf()
        with contextlib.ExitStack() as es2:
            maskf = sb(nc, es2, "g_maskf", [P, P], F32)
            maskb = sb(nc, es2, "g_maskb", [P, P], F32)
            ones = sb(nc, es2, "g_ones", [P, P], F32)
            S.dma("sp", lambda q: q.dma_start(out=maskf[:], in_=k.mask_f[:, :]), writes=[cst])
            S.dma("sp", lambda q: q.dma_start(out=maskb[:], in_=k.mask_b[:, :]), writes=[cst])
            S.op("dve", lambda v: v.memset(ones[:], 1.0), writes=[cst])
            cs_ = [sb(nc, es2, "g_cs%d" % i, [P, 2, 512], F32) for i in range(2)]
            csb = [Buf(), Buf()]
            aup = sb(nc, es2, "g_aup", [16, 2, 512], BF16)
            S.dma("pool", lambda q: q.dma_start(out=aup[:], in_=k.gla_alpha_up[ir].rearrange("d r n -> r d n")), writes=[cst])
            negab = sb(nc, es2, "g_negab", [P, 2, 4], F32)
            gg = sb(nc, es2, "g_gg", [P, 2], F32)
            with nc.allow_non_contiguous_dma(reason="tiny per-feature vectors"):
                S.dma("sp", lambda q: q.dma_start(out=negab[:], in_=k.gla_alpha_b[ir].rearrange("d (h p) -> p d h", p=P)), writes=[cst])
                S.dma("sp", lambda q: q.dma_start(out=gg[:], in_=k.gla_norm_g[ir].rearrange("(c p) -> p c", p=P)), writes=[cst])
            S.op("dve", lambda v: v.tensor_scalar(negab[:], negab[:], -1.0, None, op0=ALU.mult), writes=[cst])
            wza = sb(nc, es2, "g_wza", [P, NCH, 32], BF16)
            wzab = Buf()
            S.dma("pool", lambda q: q.dma_start(out=wza[:], in_=k.rec_w_in[ir, :, C_ZA:C_ZA + 32].rearrange("(c p) n -> p c n", p=P)),
                  writes=[wzab])
            zaT = [sb(nc, es2, "g_zaT%d" % d_, [16, TS], BF16) for d_ in range(2)]
            zab = Buf()
            for d_ in range(2):
                def ev_za(pq, pqb, n0, n1, bi, d_=d_):
                    S.op("act", lambda a: a.activation(out=zaT[d_][:, n0:n1], in_=pq[0:16, 0:n1 - n0], func=AF.Copy), reads=[pqb], writes=[zab])
                proj_fm(S, PS, HT, HTb, wza, wzab, lambda c, d_=d_: wza[:, c, d_ * 16:(d_ + 1) * 16], ev_za, m=16)
            wqk = sb(nc, es2, "g_wqk", [P, NCH, 4, P], BF16)
            wqkb = Buf()
            wvgb = wqkb
            q_r = sb(nc, es2, "g_qr", [P, TS], F32)
            k_r = sb(nc, es2, "g_kr", [P, TS], F32)
            qkb = Buf()
            sq = [sb(nc, es2, "g_sq%d" % i, [P, 2, 512], F32) for i in range(2)]
            sqb = [Buf(), Buf()]
            tmpb_ = [sq[i][:, 0, :] for i in range(2)]
            tmpbb = sqb
            Vh = sb(nc, es2, "g_V", [P, NCK, 256], BF16)
            Vb = Buf()
            Lc = sb(nc, es2, "g_Lc", [P, TS], F32)
            Lcb = Buf()
            Et = sb(nc, es2, "g_Et", [P, TS], F32)
            Etb = Buf()
            qd = sb(nc, es2, "g_qd", [P, TS], BF16)
            ki = sb(nc, es2, "g_ki", [P, TS], BF16)
            ke = Et
            dkb = Buf()
            cend = sb(nc, es2, "g_cend", [P, 2, NCK], F32)
            cendb = Buf()
            OT = sb(nc, es2, "g_OT", [P, 2, TS], F32)
            OTb = Buf()
            SG = [sb(nc, es2, "g_SG%d" % i, [P, 2, 512], BF16) for i in range(2)]
            SGb = [Buf(), Buf()]
            state = sb(nc, es2, "g_state", [P, 256], F32)
            stbf = sb(nc, es2, "g_stbf", [P, 256], BF16)
            stateb = Buf()
            stbfb = Buf()
            sTm = [sb(nc, es2, "g_sTm%d" % i, [P, P], BF16) for i in range(3)]
            sTmb = [Buf() for _ in range(3)]
            keT = [sb(nc, es2, "g_keT%d" % i, [P, P], BF16) for i in range(3)]
            keTb = [Buf() for _ in range(3)]
            rst = [sb(nc, es2, "g_rst%d" % i, [P, 512], F32) for i in range(2)]
            rstb = [Buf(), Buf()]
            YTc = [qd, ki]
            YTcb = [dkb, dkb]
            for hd in range(4):
                S.dma("pool", lambda q: q.dma_start(out=wqk[:, :, 0, :], in_=k.rec_w_in[ir, :, C_Q + hd * P:C_Q + (hd + 1) * P].rearrange("(c p) n -> p c n", p=P)), writes=[wqkb])
                S.dma("pool", lambda q: q.dma_start(out=wqk[:, :, 1, :], in_=k.rec_w_in[ir, :, C_K + hd * P:C_K + (hd + 1) * P].rearrange("(c p) n -> p c n", p=P)), writes=[wqkb])
                for a_ in range(2):
                    S.op("dve", lambda v: v.tensor_copy(out=wqk[:, :, 2 + a_, 0:P:2], in_=wqk[:, :, a_, 1:P:2]), reads=[wqkb], writes=[wqkb])
                    S.op("dve", lambda v: v.tensor_copy(out=wqk[:, :, 2 + a_, 1:P:2], in_=wqk[:, :, a_, 0:P:2]), reads=[wqkb], writes=[wqkb])
                for a_, dst in ((0, q_r), (1, k_r)):
                    for bi, (n0, n1) in enumerate(REC_BLOCKS):
                        pq, pqb = PS.get()
                        for c in range(NCH):
                            S.op("pe", lambda pe: pe.matmul(pq[:, 0:n1 - n0], lhsT=wqk[:, c, a_, :], rhs=HT[:, c, n0:n1],
                                                            start=(c == 0), stop=(c == NCH - 1)), reads=[wqkb, HTb], writes=[pqb])
                        if bi == 0:
                            S.op("act", lambda a: a.activation(out=dst[:, n0:n1], in_=pq[:, 0:n1 - n0], func=AF.Copy), reads=[pqb], writes=[qkb])
                            continue
                        ps_, psb_ = PS.get()
                        for c in range(NCH):
                            S.op("pe", lambda pe: pe.matmul(ps_[:, 0:n1 - n0], lhsT=wqk[:, c, 2 + a_, :], rhs=HT[:, c, n0:n1],
                                                            start=(c == 0), stop=(c == NCH - 1)), reads=[wqkb, HTb], writes=[psb_])
                        t0 = n0 - TC
                        u = bi % 2
                        S.dma("sp", lambda q: q.dma_start(out=cs_[u][:, 0, :], in_=k.rope_cos[:, t0:t0 + 512]), writes=[csb[u]])
                        S.dma("sp", lambda q: q.dma_start(out=cs_[u][:, 1, :], in_=k.rope_sin[:, t0:t0 + 512]), writes=[csb[u]])
                        S.op("dve", lambda v: v.tensor_tensor(out=tmpb_[u], in0=ps_[:, 0:512], in1=cs_[u][:, 1, :], op=ALU.mult),
                             reads=[psb_, csb[u]], writes=[tmpbb[u]])
                        S.op("dve", lambda v: v.tensor_tensor(out=dst[:, n0:n1], in0=pq[:, 0:512], in1=cs_[u][:, 0, :], op=ALU.mult),
                             reads=[pqb, csb[u]], writes=[qkb])
                        S.op("pool", lambda g: g.tensor_tensor(out=dst[:, n0:n1], in0=dst[:, n0:n1], in1=tmpb_[u], op=ALU.add),
                             reads=[tmpbb[u]], writes=[qkb])
                S.dma("pool", lambda q: q.dma_start(out=wqk[:, :, 0:2, :], in_=k.rec_w_in[ir, :, C_V + hd * 256:C_V + (hd + 1) * 256].rearrange("(c p) (a n) -> p c a n", p=P, a=2)), writes=[wvgb])
                S.dma("pool", lambda q: q.dma_start(out=wqk[:, :, 2:4, :], in_=k.rec_w_in[ir, :, C_G + hd * 256:C_G + (hd + 1) * 256].rearrange("(c p) (a n) -> p c a n", p=P, a=2)), writes=[wvgb])
                if hd == 0 and getattr(k, "dbg", None):
                    S.dma("sp", lambda q: q.dma_start(out=k.dbg["qr"][:, :], in_=q_r[:, :]), reads=[qkb])
                    S.dma("sp", lambda q: q.dma_start(out=k.dbg["kr"][:, :], in_=k_r[:, :]), reads=[qkb])
                for tg in range(0, NCK, 2):
                    pv, pvb = PS.get()
                    for t2 in range(2):
                        t = tg + t2
                        for c in range(NCH):
                            S.op("pe", lambda pe: pe.matmul(pv[:, t2 * 256:(t2 + 1) * 256], lhsT=HT[:, c, t * P:(t + 1) * P], rhs=wqk[:, c, 0:2, :],
                                                            start=(c == 0), stop=(c == NCH - 1)), reads=[wvgb, HTb], writes=[pvb])
                    S.op("act", lambda a: a.activation(out=Vh[:, tg:tg + 2, :], in_=pv[:, :].rearrange("p (t d) -> p t d", t=2), func=AF.Copy),
                         reads=[pvb], writes=[Vb])
                for dr in range(2):
                    for bi, (n0, n1) in enumerate(REC_BLOCKS):
                        pz, pzb = PS.get()
                        S.op("pe", lambda pe: pe.matmul(pz[:, 0:n1 - n0], lhsT=aup[:, dr, hd * P:(hd + 1) * P], rhs=zaT[dr][:, n0:n1],
                                                        start=True, stop=True), reads=[cst, zab], writes=[pzb])
                        S.op("act", lambda a: a.activation(out=Lc[:, n0:n1], in_=pz[:, 0:n1 - n0], func=AF.Exp, scale=-1.0,
                                                           bias=negab[:, dr, hd:hd + 1]), reads=[pzb, cst], writes=[Lcb])
                    S.op("act", lambda a: a.activation(out=Lc[:, :], in_=Lc[:, :], func=AF.Ln, bias=1.0, scale=1.0), reads=[Lcb], writes=[Lcb])
                    for n in range(NCK):
                        sl = slice(n * P, (n + 1) * P)
                        if dr == 0:
                            S.op("dve", lambda v: v.tensor_tensor_scan(out=Lc[:, sl], data0=ones[:, :], data1=Lc[:, sl], initial=0.0,
                                                                       op0=ALU.mult, op1=ALU.add), reads=[Lcb, cst], writes=[Lcb])
                        else:
                            rs_ = slice((n + 1) * P - 1, (n * P - 1) if n > 0 else None, -1)
                            S.op("dve", lambda v: v.tensor_tensor_scan(out=Lc[:, rs_], data0=ones[:, :], data1=Lc[:, rs_], initial=0.0,
                                                                       op0=ALU.mult, op1=ALU.add), reads=[Lcb, cst], writes=[Lcb])
                    if hd == 0 and getattr(k, "dbg", None):
                        S.dma("sp", lambda q: q.dma_start(out=k.dbg["lc%d" % dr][:, :], in_=Lc[:, :]), reads=[Lcb])
                    ce_src = Lc[:, P - 1:TS:P] if dr == 0 else Lc[:, 0:TS:P]
                    S.op("dve", lambda v: v.tensor_scalar(cend[:, 0, :], ce_src, -1.0 / 16.0, None, op0=ALU.mult), reads=[Lcb], writes=[cendb])
                    S.op("act", lambda a: a.activation(out=cend[:, 1, :], in_=cend[:, 0, :], func=AF.Exp), reads=[cendb], writes=[cendb])
                    S.op("act", lambda a: a.activation(out=Et[:, :], in_=Lc[:, :], func=AF.Exp, scale=-1.0 / 16.0), reads=[Lcb], writes=[Etb])
                    S.op("dve", lambda v: v.scalar_tensor_tensor(out=qd[:, :], in0=q_r[:, :], scalar=float(128 ** -0.5), in1=Et[:, :],
                                                                 op0=ALU.mult, op1=ALU.mult), reads=[qkb, Etb], writes=[dkb])
                    S.op("act", lambda a: a.activation(out=Et[:, :], in_=Lc[:, :], func=AF.Exp, scale=1.0 / 16.0), reads=[Lcb, dkb], writes=[Etb])
                    S.op("dve", lambda v: v.tensor_tensor(out=ki[:, :], in0=k_r[:, :], in1=Et[:, :], op=ALU.mult), reads=[qkb, Etb], writes=[dkb])
                    for n in range(NCK):
                        sl = slice(n * P, (n + 1) * P)
                        S.op("act", lambda a: a.activation(out=Et[:, sl], in_=Lc[:, sl], func=AF.Exp, scale=1.0 / 16.0, bias=cend[:, 0, n:n + 1]),
                             reads=[Lcb, cendb, dkb], writes=[Etb])
                    S.op("pool", lambda g: g.tensor_tensor(out=ke[:, :], in0=k_r[:, :], in1=Et[:, :], op=ALU.mult), reads=[qkb], writes=[Etb])
                    S.op("dve", lambda v: v.memset(state[:], 0.0), writes=[stateb])
                    order = list(range(NCK)) if dr == 0 else [1, 0] + list(range(NCK - 1, 1, -1))
                    mask = maskf if dr == 0 else maskb
                    def make_chunk(idx, n, dr=dr, mask=mask):
                        sl = slice(n * P, (n + 1) * P)
                        u = idx % 3
                        hold = {}
                        last = idx == NCK - 1

                        def c1():
                            pss, pssb = PS.get()
                            S.op("pe", lambda pe: pe.matmul(pss[:, 0:P], lhsT=ki[:, sl], rhs=qd[:, sl], start=True, stop=True), reads=[dkb], writes=[pssb])
                            S.op("dve", lambda v: v.tensor_tensor(out=sTm[u][:, :], in0=pss[:, 0:P], in1=mask[:, :], op=ALU.mult),
                                 reads=[pssb, cst], writes=[sTmb[u]])
                            if not last:
                                pt, ptb = PS.get()
                                S.op("pe", lambda pe: pe.transpose(out=pt[:, 0:P], in_=ke[:, sl], identity=ident[:]), reads=[Etb, identb], writes=[ptb])
                                S.op("act", lambda a: a.activation(out=keT[u][:, :], in_=pt[:, 0:P], func=AF.Copy), reads=[ptb], writes=[keTb[u]])

                        def c2_():
                            po, pob = PS.get()
                            hold["po"] = (po, pob)
                            for c2 in range(2):
                                S.op("pe", lambda pe: pe.matmul(po[:, c2 * P:(c2 + 1) * P], lhsT=Vh[:, n, c2 * P:(c2 + 1) * P], rhs=sTm[u][:, :],
                                                                start=(c2 == 0), stop=(idx == 0 and c2 == 1)), reads=[Vb, sTmb[u]], writes=[pob])
                            if not last:
                                pkv, pkvb = PSK.get()
                                hold["pkv"] = (pkv, pkvb)
                                S.op("pe", lambda pe: pe.matmul(pkv[:, 0:256], lhsT=keT[u][:, :], rhs=Vh[:, n, :], start=True, stop=True),
                                     reads=[keTb[u], Vb], writes=[pkvb])

                        def c3():
                            po, pob = hold["po"]
                            if idx > 0:
                                for c2 in range(2):
                                    S.op("pe", lambda pe: pe.matmul(po[:, c2 * P:(c2 + 1) * P], lhsT=stbf[:, c2 * P:(c2 + 1) * P], rhs=qd[:, sl],
                                                                    start=False, stop=(c2 == 1)), reads=[stbfb, dkb], writes=[pob])
                            src_ = po[:, 0:256].rearrange("p (c i) -> p c i", c=2)
                            if dr == 0:
                                S.op("act", lambda a: a.activation(out=OT[:, :, sl], in_=src_, func=AF.Copy), reads=[pob], writes=[OTb])
                            else:
                                S.op("dve", lambda v: v.tensor_tensor(out=OT[:, :, sl], in0=src_, in1=OT[:, :, sl], op=ALU.add), reads=[pob], writes=[OTb])

                        def c4():
                            if last:
                                return
                            pkv, pkvb = hold["pkv"]
                            S.op("dve", lambda v: v.scalar_tensor_tensor(out=state[:, :], in0=state[:, :], scalar=cend[:, 1, n:n + 1], in1=pkv[:, 0:256],
                                                                         op0=ALU.mult, op1=ALU.add), reads=[pkvb, cendb], writes=[stateb])
                            S.op("act", lambda a: a.activation(out=stbf[:, :], in_=state[:, :], func=AF.Copy), reads=[stateb], writes=[stbfb])
                        return c1, c2_, c3, c4

                    chunks = [make_chunk(idx, n) for idx, n in enumerate(order)]
                    for step in range(NCK + 3):
                        if 0 <= step - 3 < NCK:
                            chunks[step - 3][3]()
                        if step < NCK:
                            chunks[step][0]()
                        if 0 <= step - 1 < NCK:
                            chunks[step - 1][1]()
                        if 0 <= step - 2 < NCK:
                            chunks[step - 2][2]()
                if hd == 0 and getattr(k, "dbg", None):
                    S.dma("sp", lambda q: q.dma_start(out=k.dbg["ot"][:, :, :], in_=OT[:, :, :]), reads=[OTb])
                    S.dma("sp", lambda q: q.dma_start(out=k.dbg["vh"][:, :, :], in_=Vh[:, :, :]), reads=[Vb])
                for bi, (n0, n1) in enumerate(REC_BLOCKS):
                    u = bi % 2
                    w_ = n1 - n0
                    for c2 in range(2):
                        pg_, pgb_ = PS.get()
                        for c in range(NCH):
                            S.op("pe", lambda pe: pe.matmul(pg_[:, 0:w_], lhsT=wqk[:, c, 2 + c2, :], rhs=HT[:, c, n0:n1],
                                                            start=(c == 0), stop=(c == NCH - 1)), reads=[wvgb, HTb], writes=[pgb_])
                        S.op("act", lambda a: a.activation(out=SG[u][:, c2, 0:w_], in_=pg_[:, 0:w_], func=AF.Silu), reads=[pgb_], writes=[SGb[u]])
                    S.op("act", lambda a: a.activation(out=sq[u][:, :, 0:w_], in_=OT[:, :, n0:n1], func=AF.Square), reads=[OTb], writes=[sqb[u]])
                    pr, prb = PS.get()
                    for c2 in range(2):
                        S.op("pe", lambda pe: pe.matmul(pr[:, 0:w_], lhsT=ones[:, :], rhs=sq[u][:, c2, 0:w_], start=(c2 == 0), stop=(c2 == 1)),
                             reads=[cst, sqb[u]], writes=[prb])
                    S.op("dve", lambda v: v.tensor_scalar(rst[u][:, 0:w_], pr[:, 0:w_], 1.0 / 256.0, EPS, op0=ALU.mult, op1=ALU.add),
                         reads=[prb], writes=[rstb[u]])
                    S.op("act", lambda a: a.activation(out=rst[u][:, 0:w_], in_=rst[u][:, 0:w_], func=AF.Sqrt), reads=[rstb[u]], writes=[rstb[u]])
                    S.op("dve", lambda v: v.reciprocal(rst[u][:, 0:w_], rst[u][:, 0:w_]), reads=[rstb[u]], writes=[rstb[u]])
                    for c2 in range(2):
                        S.op("dve", lambda v: v.scalar_tensor_tensor(out=sq[u][:, c2, 0:w_], in0=OT[:, c2, n0:n1], scalar=gg[:, c2:c2 + 1], in1=rst[u][:, 0:w_],
                                                                     op0=ALU.mult, op1=ALU.mult), reads=[OTb, cst, rstb[u]], writes=[sqb[u]])
                        S.op("pool", lambda g: g.tensor_tensor(out=YTc[c2][:, n0:n1], in0=sq[u][:, c2, 0:w_], in1=SG[u][:, c2, 0:w_], op=ALU.mult),
                             reads=[sqb[u], SGb[u]], writes=[YTcb[c2]])
                for c2 in range(2):
                    S.dma("sp", lambda q: q.dma_start(out=k.YTd[hd * 2 + c2, :, :], in_=YTc[c2][:, :]), reads=[YTcb[c2]])
        S.barrier()
        with contextlib.ExitStack() as es3:
            wa = sb(nc, es3, "l_wa", [P, 2, 8, P], F32)
            wi_ = sb(nc, es3, "l_wi", [P, 2, 8, P], F32)
            S.dma("sp", lambda q: q.dma_start(out=wa[:], in_=k.lru_w_a[ir].rearrange("r n d e -> d r n e")), writes=[cst])
            S.dma("sp", lambda q: q.dma_start(out=wi_[:], in_=k.lru_w_i[ir].rearrange("r n d e -> d r n e")), writes=[cst])
            cw = sb(nc, es3, "l_cw", [P, 4, 8], F32)
            cbias = sb(nc, es3, "l_cb", [P, 8], F32)
            ba = sb(nc, es3, "l_ba", [P, 2, 8], F32)
            bi_ = sb(nc, es3, "l_bi", [P, 2, 8], F32)
            lam = sb(nc, es3, "l_lam", [P, 2, 8], F32)
            with nc.allow_non_contiguous_dma(reason="tiny per-feature vectors"):
                S.dma("sp", lambda q: q.dma_start(out=cw[:], in_=k.lru_conv_w[ir].rearrange("j (n p) -> p j n", p=P)), writes=[cst])
                S.dma("sp", lambda q: q.dma_start(out=cbias[:], in_=k.lru_conv_b[ir].rearrange("(n p) -> p n", p=P)), writes=[cst])
                S.dma("sp", lambda q: q.dma_start(out=ba[:], in_=k.lru_b_a[ir].rearrange("r (n p) -> p r n", p=P)), writes=[cst])
                S.dma("sp", lambda q: q.dma_start(out=bi_[:], in_=k.lru_b_i[ir].rearrange("r (n p) -> p r n", p=P)), writes=[cst])
                S.dma("sp", lambda q: q.dma_start(out=lam[:], in_=k.lru_lambda[ir].rearrange("r (n p) -> p r n", p=P)), writes=[cst])
            S.op("act", lambda a: a.activation(out=lam[:], in_=lam[:], func=AF.Exp, scale=-1.0), reads=[cst], writes=[cst])
            S.op("act", lambda a: a.activation(out=lam[:], in_=lam[:], func=AF.Ln, bias=1.0, scale=1.0), reads=[cst], writes=[cst])
            S.op("dve", lambda v: v.tensor_scalar(lam[:], lam[:], -8.0, None, op0=ALU.mult), reads=[cst], writes=[cst])
            wx0 = sb(nc, es3, "l_wx0", [P, NCH, 2, P], BF16)
            wx = [wx0, wx0]
            wxb0 = Buf()
            wxb = [wxb0, wxb0]
            names = ["xr", "xc", "GG", "R0", "I0", "A0", "R1", "I1", "A1", "H0", "H1"]
            T_ = {n_: sb(nc, es3, "l_" + n_, [P, TS], F32) for n_ in names}
            B_ = {n_: Buf() for n_ in names}
            yb = [sb(nc, es3, "l_y%d" % i, [P, TS], BF16) for i in range(1)]
            ybb = [Buf()]
            lam2 = sb(nc, es3, "l_lam2", [P, 2, 8], F32)
            S.op("dve", lambda v: v.tensor_scalar(lam2[:], lam[:], 2.0, None, op0=ALU.mult), reads=[cst], writes=[cst])
            for bk in range(8):
                j = bk % 2

                S.dma("pool", lambda q: q.dma_start(out=wx[j][:, :, 0, :], in_=k.rec_w_in[ir, :, C_LX + bk * P:C_LX + (bk + 1) * P].rearrange("(c p) n -> p c n", p=P)), writes=[wxb[j]])
                S.dma("pool", lambda q: q.dma_start(out=wx[j][:, :, 1, :], in_=k.rec_w_in[ir, :, C_LG + bk * P:C_LG + (bk + 1) * P].rearrange("(c p) n -> p c n", p=P)), writes=[wxb[j]])

                def ev_x(pq, pqb, n0, n1, bi):
                    S.op("act", lambda a: a.activation(out=T_["xr"][:, n0:n1], in_=pq[:, 0:n1 - n0], func=AF.Copy), reads=[pqb], writes=[B_["xr"]])

                def ev_gt(pq, pqb, n0, n1, bi):
                    S.op("act", lambda a: a.activation(out=T_["GG"][:, n0:n1], in_=pq[:, 0:n1 - n0], func=AF.Copy), reads=[pqb], writes=[B_["GG"]])
                proj_fm(S, PS, HT, HTb, wx[j], wxb[j], lambda c: wx[j][:, c, 0, :], ev_x)
                proj_fm(S, PS, HT, HTb, wx[j], wxb[j], lambda c: wx[j][:, c, 1, :], ev_gt)
                GG, xr, xc = T_["GG"], T_["xr"], T_["xc"]
                for (a_, b_) in ((0, TC), (TC, TS)):
                    S.op("dve", lambda v: v.tensor_scalar(xc[:, a_:b_], xr[:, a_:b_], cw[:, 1, bk:bk + 1], cbias[:, bk:bk + 1], op0=ALU.mult, op1=ALU.add),
                         reads=[B_["xr"], cst], writes=[B_["xc"]])
                    S.op("dve", lambda v: v.scalar_tensor_tensor(out=xc[:, a_ + 1:b_], in0=xr[:, a_:b_ - 1], scalar=cw[:, 0, bk:bk + 1], in1=xc[:, a_ + 1:b_],
                                                                 op0=ALU.mult, op1=ALU.add), reads=[B_["xr"], cst], writes=[B_["xc"]])
                    S.op("dve", lambda v: v.scalar_tensor_tensor(out=xc[:, a_:b_ - 1], in0=xr[:, a_ + 1:b_], scalar=cw[:, 2, bk:bk + 1], in1=xc[:, a_:b_ - 1],
                                                                 op0=ALU.mult, op1=ALU.add), reads=[B_["xr"], cst], writes=[B_["xc"]])
                    S.op("dve", lambda v: v.scalar_tensor_tensor(out=xc[:, a_:b_ - 2], in0=xr[:, a_ + 2:b_], scalar=cw[:, 3, bk:bk + 1], in1=xc[:, a_:b_ - 2],
                                                                 op0=ALU.mult, op1=ALU.add), reads=[B_["xr"], cst], writes=[B_["xc"]])
                for bi, (n0, n1) in enumerate(REC_BLOCKS):
                    for dr in range(2):
                        R_, I_ = T_["R%d" % dr], T_["I%d" % dr]
                        pr, prb = PS.get()
                        S.op("pe", lambda pe: pe.matmul(pr[:, 0:n1 - n0], lhsT=wa[:, dr, bk, :], rhs=xc[:, n0:n1], start=True, stop=True),
                             reads=[cst, B_["xc"]], writes=[prb])
                        S.op("act", lambda a: a.activation(out=R_[:, n0:n1], in_=pr[:, 0:n1 - n0], func=AF.Sigmoid, bias=ba[:, dr, bk:bk + 1], scale=1.0),
                             reads=[prb, cst], writes=[B_["R%d" % dr]])
                        pi, pib = PS.get()
                        S.op("pe", lambda pe: pe.matmul(pi[:, 0:n1 - n0], lhsT=wi_[:, dr, bk, :], rhs=xc[:, n0:n1], start=True, stop=True),
                             reads=[cst, B_["xc"]], writes=[pib])
                        S.op("act", lambda a: a.activation(out=I_[:, n0:n1], in_=pi[:, 0:n1 - n0], func=AF.Sigmoid, bias=bi_[:, dr, bk:bk + 1], scale=1.0),
                             reads=[pib, cst], writes=[B_["I%d" % dr]])
                S.op("pool", lambda g: g.tensor_tensor(out=xr[:, :], in0=GG[:, :], in1=GG[:, :], op=ALU.mult), reads=[B_["GG"], B_["xc"]], writes=[B_["xr"]])
                S.op("dve", lambda v: v.tensor_scalar(xr[:, :], xr[:, :], 0.044715, 1.0, op0=ALU.mult, op1=ALU.add), writes=[B_["xr"]])
                S.op("pool", lambda g: g.tensor_tensor(out=xr[:, :], in0=xr[:, :], in1=GG[:, :], op=ALU.mult), reads=[B_["GG"]], writes=[B_["xr"]])
                S.op("act", lambda a: a.activation(out=xr[:, :], in_=xr[:, :], func=AF.Sigmoid, scale=1.5957691216), writes=[B_["xr"]])
                S.op("dve", lambda v: v.tensor_tensor(out=GG[:, :], in0=GG[:, :], in1=xr[:, :], op=ALU.mult), reads=[B_["xr"]], writes=[B_["GG"]])
                for dr in range(2):
                    R_, I_, A_ = T_["R%d" % dr], T_["I%d" % dr], T_["A%d" % dr]
                    Rb, Ib, Ab = B_["R%d" % dr], B_["I%d" % dr], B_["A%d" % dr]
                    S.op("act", lambda a: a.activation(out=A_[:, :], in_=R_[:, :], func=AF.Exp, scale=lam[:, dr, bk:bk + 1]), reads=[Rb, cst], writes=[Ab])
                    S.op("act", lambda a: a.activation(out=R_[:, :], in_=R_[:, :], func=AF.Exp, scale=lam2[:, dr, bk:bk + 1]), reads=[cst], writes=[Rb])
                    S.op("pool", lambda g: g.tensor_tensor(out=I_[:, :], in0=I_[:, :], in1=xc[:, :], op=ALU.mult), reads=[B_["xc"]], writes=[Ib])
                for dr in range(2):
                    R_, I_ = T_["R%d" % dr], T_["I%d" % dr]
                    Rb, Ib = B_["R%d" % dr], B_["I%d" % dr]
                    S.op("act", lambda a: a.activation(out=R_[:, :], in_=R_[:, :], func=AF.Sqrt, scale=-1.0, bias=1.0), writes=[Rb])
                    S.op("dve", lambda v: v.tensor_tensor(out=R_[:, :], in0=R_[:, :], in1=I_[:, :], op=ALU.mult), reads=[Ib], writes=[Rb])
                H0, H1 = T_["H0"], T_["H1"]
                S.op("dve", lambda v: v.tensor_tensor_scan(out=H0[:, :], data0=T_["A0"][:, :], data1=T_["R0"][:, :], initial=0.0, op0=ALU.mult, op1=ALU.add),
                     reads=[B_["A0"], B_["R0"]], writes=[B_["H0"]])
                S.op("dve", lambda v: v.tensor_tensor_scan(out=H1[:, TC - 1::-1], data0=T_["A1"][:, TC - 1::-1], data1=T_["R1"][:, TC - 1::-1], initial=0.0,
                                                           op0=ALU.mult, op1=ALU.add), reads=[B_["A1"], B_["R1"]], writes=[B_["H1"]])
                S.op("dve", lambda v: v.tensor_tensor_scan(out=H1[:, TS - 1:TC - 1:-1], data0=T_["A1"][:, TS - 1:TC - 1:-1], data1=T_["R1"][:, TS - 1:TC - 1:-1],
                                                           initial=H1[:, 0:1], op0=ALU.mult, op1=ALU.add), reads=[B_["A1"], B_["R1"]], writes=[B_["H1"]])
                S.op("pool", lambda g: g.tensor_tensor(out=H0[:, :], in0=H0[:, :], in1=H1[:, :], op=ALU.add), reads=[B_["H1"]], writes=[B_["H0"]])
                j = 0
                S.op("dve", lambda v: v.tensor_tensor(out=yb[j][:, :], in0=H0[:, :], in1=GG[:, :], op=ALU.mult), reads=[B_["H0"], B_["GG"]], writes=[ybb[j]])
                S.dma("sp", lambda q: q.dma_start(out=k.YTd[8 + bk, :, :], in_=yb[j][:, :]), reads=[ybb[j]])
    S.barrier()


def final_phase(k, S):
    nc = k.nc
    with contextlib.ExitStack() as es:
        xts = [sb(nc, es, "fn_xt%d" % i, [P, D], F32) for i in range(3)]
        xbs = [Buf() for _ in range(3)]
        hms = [sb(nc, es, "fn_hm%d" % i, [P, D], F32) for i in range(3)]
        hbs = [Buf() for _ in range(3)]
        sm = [sb(nc, es, "fn_sm%d" % i, [P, 8], F32) for i in range(3)]
        smb = [Buf() for _ in range(3)]
        grow = sb(nc, es, "fn_g", [P, D], F32)
        gb = Buf()
        S.dma("sp", lambda q: q.dma_start(out=grow[:], in_=k.norm_f_g[0:1, :].partition_broadcast(P)), writes=[gb])
        it = 0
        tiles_st = []
        for s in range(NS):
            for t in range(TL // P):
                j = it % 3
                it += 1
                r0 = s * TS + TC + t * P

                def fA(j=j, r0=r0):
                    S.dma("sp", lambda q: q.dma_start(out=xts[j][:, :], in_=k.X[r0:r0 + P, :]), writes=[xbs[j]])
                    S.op("act", lambda a: a.activation(out=hms[j][:, :], in_=xts[j][:, :], func=AF.Square, accum_out=sm[j][:, 0:1]),
                         reads=[xbs[j]], writes=[hbs[j], smb[j]])

                def fB(j=j):
                    small = sm[j]
                    S.op("dve", lambda v: v.tensor_scalar(small[:, 1:2], small[:, 0:1], 1.0 / D, EPS, op0=ALU.mult, op1=ALU.add),
                         reads=[smb[j]], writes=[smb[j]])
                    S.op("act", lambda a: a.activation(out=small[:, 2:3], in_=small[:, 1:2], func=AF.Sqrt), reads=[smb[j]], writes=[smb[j]])
                    S.op("dve", lambda v: v.reciprocal(small[:, 3:4], small[:, 2:3]), reads=[smb[j]], writes=[smb[j]])
                    S.op("dve", lambda v: v.scalar_tensor_tensor(out=hms[j][:, :], in0=xts[j][:, :], scalar=small[:, 3:4], in1=grow[:, :],
                                                                 op0=ALU.mult, op1=ALU.mult), reads=[xbs[j], smb[j], gb], writes=[hbs[j]])

                def fC(j=j, s=s, t=t):
                    S.dma("sp", lambda q: q.dma_start(out=k.out[s, t * P:(t + 1) * P, :], in_=hms[j][:, :]), reads=[hbs[j]])
                tiles_st.append([fA, fB, fC])
        run_pipelined(tiles_st)
    S.barrier()


WEIGHT_SHAPES = {
    "w_mod": [DEPTH, D, 6 * D], "b_mod": [DEPTH, 6 * D], "norm_mix_g": [DEPTH, D], "norm_ffn_g": [DEPTH, D],
    "w_out": [DEPTH, D, D], "router_w": [DEPTH, D, NE], "exp_w_gate": [DEPTH, NE, D, FF], "exp_w_up": [DEPTH, NE, D, FF],
    "exp_w_down": [DEPTH, NE, FF, D], "rec_w_in": [2, D, 5152], "gla_alpha_up": [2, 2, 16, 512], "gla_alpha_b": [2, 2, 512],
    "gla_norm_g": [2, 256], "lru_conv_w": [2, 4, 1024], "lru_conv_b": [2, 1024], "lru_w_a": [2, 2, 8, P, P],
    "lru_b_a": [2, 2, 1024], "lru_w_i": [2, 2, 8, P, P], "lru_b_i": [2, 2, 1024], "lru_lambda": [2, 2, 1024],
    "na_w_qkv": [2, D, 3 * D], "norm_f_g": [1, D],
}
CONST_SHAPES = {"ident_d": [P, P], "mask_f": [P, P], "mask_b": [P, P], "rope_cos": [P, TL], "rope_sin": [P, TL],
                "na_tab": [2, 16, 5, P, 640]}


def build_program():
    nc = bass.Bass("TRN2", target_bir_lowering=False)
    k = Ctx()
    k.nc = nc

    def inp(name, shape):
        return nc.dram_tensor(name, shape, F32, kind="ExternalInput").ap()
    k.x_in = inp("x", [NS, TL, D])
    k.ctx_in = inp("ctx", [NS, TC, D])
    k.c = inp("c", [NS, D])
    k.c_ctx = inp("c_ctx", [1, D])
    for n_, sh in WEIGHT_SHAPES.items():
        setattr(k, n_, inp(n_, sh))
    for n_, sh in CONST_SHAPES.items():
        setattr(k, n_, inp(n_, sh))
    k.out = nc.dram_tensor("out", [NS, TL, D], F32, kind="ExternalOutput").ap()
    k.X = nc.dram_tensor("scr_X", [NS * TS, D], F32, kind="Internal").ap()
    k.H2 = nc.dram_tensor("scr_H2", [NS * TS, D], F32, kind="Internal").ap()
    k.MODR = nc.dram_tensor("scr_MODR", [DEPTH, 3, 6, D], F32, kind="Internal").ap()
    k.YTd = nc.dram_tensor("scr_YT", [NCH, P, TS], BF16, kind="Internal").ap()
    with contextlib.ExitStack() as es:
        S = Sched(nc, es)
        xb = Buf()
        for s in range(NS):
            S.dma("sp", lambda q: q.dma_start(out=k.X[s * TS:s * TS + TC, :], in_=k.ctx_in[s, :, :]), writes=[xb])
            for hh in range(4):
                S.dma("sp", lambda q: q.dma_start(out=k.X[s * TS + TC + hh * 512:s * TS + TC + (hh + 1) * 512, :],
                                                  in_=k.x_in[s, hh * 512:(hh + 1) * 512, :]), writes=[xb])
        S.barrier()
        mod_phase(k, S)
        for l in range(DEPTH):
            last = l == DEPTH - 1
            for s in range(NS):
                if l % 2 == 0:
                    rec_phase(k, S, l, s)
                else:
                    na_phase(k, S, l, s, do_ctx=not last)
                outproj_phase(k, S, l, s, do_ctx=not last)
            ffn_phase(k, S, l, do_ctx=not last)
        final_phase(k, S)
        S.barrier()
    return nc


def kernel(**inputs):
    n_cores = 8
    f32 = lambda a: np.ascontiguousarray(np.asarray(a, dtype=np.float32))
    shared = {}
    for n_, sh in WEIGHT_SHAPES.items():
        shared[n_] = f32(inputs[n_]).reshape(sh)
    cosF, sinF = build_rope_tabs()
    shared["ident_d"] = np.eye(P, dtype=np.float32)
    shared["mask_f"] = np.triu(np.ones((P, P), np.float32))
    shared["mask_b"] = np.tril(np.ones((P, P), np.float32))
    shared["rope_cos"] = cosF
    shared["rope_sin"] = sinF
    shared["na_tab"] = build_na_tab(f32(inputs["na_rpb"]))
    shared["c_ctx"] = f32(inputs["c_ctx"]).reshape(1, D)
    x = f32(inputs["x"])
    ctx = f32(inputs["ctx"])
    c = f32(inputs["c"])
    in_maps = []
    for i in range(n_cores):
        m = dict(shared)
        m["x"] = x[i * NS:(i + 1) * NS]
        m["ctx"] = ctx[i * NS:(i + 1) * NS]
        m["c"] = c[i * NS:(i + 1) * NS]
        in_maps.append(m)
    nc = build_program()
    res = run_bass_kernel_spmd(nc, in_maps, core_ids=list(range(n_cores)))
    return np.concatenate([np.asarray(r["out"], dtype=np.float32) for r in res.results], axis=0)
```

```python
import contextlib
import numpy as np
import concourse.bass as bass
import concourse.mybir as mybir
from concourse.bass_utils import run_bass_kernel_spmd

F32 = mybir.dt.float32
BF16 = mybir.dt.bfloat16
U32 = mybir.dt.uint32
I32 = mybir.dt.int32
AF = mybir.ActivationFunctionType
ALU = mybir.AluOpType
AX = mybir.AxisListType

P = 128
D = 2048
NCH = D // P
TL = 2048
TC = 256
TS = TC + TL
NS = 2
NE = 16
FF = 1024
CAPL = 256
CAPC = 32
DEPTH = 4
EPS = 1e-6
NDS = 8
SELF_SYNC = True
NA_HEAD_BARRIER = False
NA_STAGES = 4
NA_PAIR = 2


class Buf:
    __slots__ = ("w", "r")

    def __init__(self):
        self.w = None
        self.r = {}


class Sched:
    def __init__(self, nc, es):
        self.nc = nc
        self.eng = {"pe": nc.tensor, "dve": nc.vector, "act": nc.scalar, "pool": nc.gpsimd, "sp": nc.sync}
        self.csem = {e: es.enter_context(nc.semaphore("c_" + e)) for e in ("pe", "dve", "act", "pool")}
        self.ccnt = {e: 0 for e in self.csem}
        self.dsem = {q: [es.enter_context(nc.semaphore("d_%s_%d" % (q, i))) for i in range(NDS)]
                     for q in ("sp", "pool", "act")}
        self.dcnt = {q: [0] * NDS for q in self.dsem}
        self.dnext = {q: 0 for q in self.dsem}
        self.waited = {}
        ss = SELF_SYNC
        self.self_sync = {"pe": False, "dve": ss, "act": ss, "pool": ss, "sp": True}

    def wait(self, e, ev):
        if ev is None:
            return
        sem, val, key = ev
        if key == ("c", e) and not self.self_sync[e]:
            return
        if self.waited.get((e, key), 0) >= val:
            return
        self.eng[e].wait_ge(sem, val)
        self.waited[(e, key)] = val

    def _deps(self, e, reads, writes):
        for b in reads:
            self.wait(e, b.w)
        for b in writes:
            self.wait(e, b.w)
            for ev in list(b.r.values()):
                self.wait(e, ev)

    def _mark(self, ev, reads, writes):
        for b in reads:
            old = b.r.get(ev[2])
            if old is None or old[1] < ev[1]:
                b.r[ev[2]] = ev
        for b in writes:
            b.w = ev
            b.r = {}

    def op(self, e, fn, reads=(), writes=()):
        self._deps(e, reads, writes)
        inst = fn(self.eng[e])
        self.ccnt[e] += 1
        inst.then_inc(self.csem[e], 1)
        ev = (self.csem[e], self.ccnt[e], ("c", e))
        self._mark(ev, reads, writes)
        return ev

    def dma(self, q, fn, reads=(), writes=()):
        self._deps(q, reads, writes)
        i = self.dnext[q]
        self.dnext[q] = (i + 1) % NDS
        sem = self.dsem[q][i]
        key = ("d", q, i)
        if self.dcnt[q][i] > 0:
            self.wait(q, (sem, self.dcnt[q][i], key))
        inst = fn(self.eng[q])
        self.dcnt[q][i] += 16
        inst.then_inc(sem, 16)
        ev = (sem, self.dcnt[q][i], key)
        self._mark(ev, reads, writes)
        return ev

    def barrier(self):
        evs = [(self.csem[o], self.ccnt[o], ("c", o)) for o in self.csem if self.ccnt[o] > 0]
        for q in self.dsem:
            for i in range(NDS):
                if self.dcnt[q][i] > 0:
                    evs.append((self.dsem[q][i], self.dcnt[q][i], ("d", q, i)))
        for e in ("pe", "dve", "act", "pool", "sp"):
            ss = self.self_sync[e]
            self.self_sync[e] = True
            for ev in evs:
                self.wait(e, ev)
            self.self_sync[e] = ss


class Ctx:
    pass


def run_pipelined(tiles_stages):
    n = len(tiles_stages)
    nst = max(len(s_) for s_ in tiles_stages)
    for step in range(n + nst - 1):
        for sidx in reversed(range(nst)):
            t = step - sidx
            if 0 <= t < n and sidx < len(tiles_stages[t]):
                tiles_stages[t][sidx]()


_UID = [0]


def uname(name):
    _UID[0] += 1
    return "%s_u%d" % (name, _UID[0])


def sb(nc, es, name, shape, dt):
    return es.enter_context(nc.sbuf_tensor(uname(name), shape, dt))


def norm_tile(k, S, xt, xb, hm, hb, small, smb, X, r0, grow, srow, gsb, rows=P):
    S.dma("sp", lambda q: q.dma_start(out=xt[:rows, :], in_=X[r0:r0 + rows, :]), writes=[xb])
    S.op("act", lambda a: a.activation(out=hm[:rows, :], in_=xt[:rows, :], func=AF.Square,
                                       accum_out=small[:rows, 0:1]),
         reads=[xb], writes=[hb, smb])
    S.op("dve", lambda v: v.tensor_scalar(small[:rows, 1:2], small[:rows, 0:1], 1.0 / D, EPS,
                                          op0=ALU.mult, op1=ALU.add), reads=[smb], writes=[smb])
    S.op("act", lambda a: a.activation(out=small[:rows, 2:3], in_=small[:rows, 1:2], func=AF.Sqrt),
         reads=[smb], writes=[smb])
    S.op("dve", lambda v: v.reciprocal(small[:rows, 3:4], small[:rows, 2:3]), reads=[smb], writes=[smb])
    S.op("dve", lambda v: v.scalar_tensor_tensor(out=hm[:rows, :], in0=xt[:rows, :], scalar=small[:rows, 3:4],
                                                 in1=grow[:rows, :], op0=ALU.mult, op1=ALU.mult),
         reads=[xb, smb, gsb], writes=[hb])
    S.op("pool", lambda g: g.tensor_tensor(out=hm[:rows, :], in0=hm[:rows, :], in1=srow[:rows, :], op=ALU.add),
         reads=[hb, gsb], writes=[hb])


def norm_stages(S, X, r0, xt, xb, hm, hb, small, smb, grow, srow, gsb):
    def sA():
        S.dma("sp", lambda q: q.dma_start(out=xt[:, :], in_=X[r0:r0 + P, :]), writes=[xb])
        S.op("act", lambda a: a.activation(out=hm[:, :], in_=xt[:, :], func=AF.Square, accum_out=small[:, 0:1]),
             reads=[xb], writes=[hb, smb])

    def sB():
        S.op("dve", lambda v: v.tensor_scalar(small[:, 1:2], small[:, 0:1], 1.0 / D, EPS, op0=ALU.mult, op1=ALU.add),
             reads=[smb], writes=[smb])
        S.op("act", lambda a: a.activation(out=small[:, 2:3], in_=small[:, 1:2], func=AF.Sqrt), reads=[smb], writes=[smb])
        S.op("dve", lambda v: v.reciprocal(small[:, 3:4], small[:, 2:3]), reads=[smb], writes=[smb])
        S.op("dve", lambda v: v.scalar_tensor_tensor(out=hm[:, :], in0=xt[:, :], scalar=small[:, 3:4], in1=grow[:, :],
                                                     op0=ALU.mult, op1=ALU.mult), reads=[xb, smb, gsb], writes=[hb])
        S.op("pool", lambda g: g.tensor_tensor(out=hm[:, :], in0=hm[:, :], in1=srow[:, :], op=ALU.add),
             reads=[hb, gsb], writes=[hb])
    return sA, sB


class PsumPool:
    def __init__(self, nc, es, n=8, dt=F32, width=512):
        self.tiles = [es.enter_context(nc.psum_tensor(uname("ps%d" % i), [P, width], dt)) for i in range(n)]
        self.bufs = [Buf() for _ in range(n)]
        self.i = 0
        self.n = n

    def get(self):
        i = self.i
        self.i = (i + 1) % self.n
        return self.tiles[i], self.bufs[i]


def ffn_phase(k, S, l, do_ctx=True):
    nc = k.nc
    X, H2, MODR = k.X, k.H2, k.MODR
    with contextlib.ExitStack() as es:
        PS = PsumPool(nc, es)
        ident = sb(nc, es, "f_ident", [P, P], F32)
        identb = Buf()
        S.dma("sp", lambda q: q.dma_start(out=ident[:], in_=k.ident_d[:, :]), writes=[identb])
        wr = sb(nc, es, "f_wr", [P, NCH, NE], F32)
        wrb = Buf()
        S.dma("sp", lambda q: q.dma_start(out=wr[:], in_=k.router_w[l].rearrange("(c p) e -> p c e", p=P)),
              writes=[wrb])
        idxl = sb(nc, es, "f_idxl", [P, 4, NE], I32)
        gatel = sb(nc, es, "f_gatel", [P, 4, NE], F32)
        idxc = sb(nc, es, "f_idxc", [P, NE], I32)
        gatec = sb(nc, es, "f_gatec", [P, NE], F32)
        tabb = Buf()
        with contextlib.ExitStack() as es2:
            NBN = 3
            xts = [sb(nc, es2, "fa_xt%d" % i, [P, D], F32) for i in range(NBN)]
            xbs = [Buf() for _ in range(NBN)]
            hms = [sb(nc, es2, "fa_hm%d" % i, [P, D], F32) for i in range(NBN)]
            hbs = [Buf() for _ in range(NBN)]
            smalls = [sb(nc, es2, "fa_sm%d" % i, [P, 8], F32) for i in range(NBN)]
            smbs = [Buf() for _ in range(NBN)]
            grow = sb(nc, es2, "fa_grow", [P, D], F32)
            srow = sb(nc, es2, "fa_srow", [P, D], F32)
            gnorm = sb(nc, es2, "fa_gnorm", [P, D], F32)
            gsb = Buf()
            gnb = Buf()
            hT = [sb(nc, es2, "fa_hT%d" % i, [P, NCH, P], F32) for i in range(NBN)]
            hTb = [Buf() for _ in range(NBN)]
            lg = [sb(nc, es2, "fa_lg%d" % i, [P, 64], F32) for i in range(NBN)]
            lgb = [Buf() for _ in range(NBN)]
            affl = sb(nc, es2, "fa_affl", [48, TL], F32)
            afflb = Buf()
            affc = sb(nc, es2, "fa_affc", [48, TC], F32)
            affcb = Buf()
            S.op("dve", lambda v: v.memset(affl[:], 0.0), writes=[afflb])
            S.op("dve", lambda v: v.memset(affc[:], 0.0), writes=[affcb])
            S.dma("sp", lambda q: q.dma_start(out=gnorm[:], in_=k.norm_ffn_g[l:l + 1, :].partition_broadcast(P)),
                  writes=[gnb])
            it = 0
            for s in range(NS):
                for grp in ((0, 1) if do_ctx else (1,)):
                    ms = 2 if grp == 0 else s
                    S.dma("sp", lambda q: q.dma_start(out=grow[:], in_=MODR[l, ms, 4:5, :].partition_broadcast(P)),
                          writes=[gsb])
                    S.dma("sp", lambda q: q.dma_start(out=srow[:], in_=MODR[l, ms, 3:4, :].partition_broadcast(P)),
                          writes=[gsb])
                    S.op("dve", lambda v: v.scalar_tensor_tensor(out=grow[:], in0=grow[:], scalar=1.0, in1=gnorm[:],
                                                                 op0=ALU.add, op1=ALU.mult),
                         reads=[gnb], writes=[gsb])
                    ntile = (TC if grp == 0 else TL) // P
                    rbase = s * TS + (0 if grp == 0 else TC)
                    tiles_st = []
                    for t in range(ntile):
                        j = it % NBN
                        it += 1
                        r0 = rbase + t * P
                        sA, sB = norm_stages(S, X, r0, xts[j], xbs[j], hms[j], hbs[j], smalls[j], smbs[j], grow, srow, gsb)
                        if grp == 0:
                            dst, dstb = affc[s * 32:s * 32 + NE, t * P:(t + 1) * P], affcb
                        else:
                            dst, dstb = affl[s * 32:s * 32 + NE, t * P:(t + 1) * P], afflb
                        hold = {}

                        def sC(j=j, r0=r0):
                            S.dma("sp", lambda q: q.dma_start(out=H2[r0:r0 + P, :], in_=hms[j][:, :]), reads=[hbs[j]])
                            for cb in range(4):
                                pt, pb = PS.get()
                                for c4 in range(4):
                                    c = cb * 4 + c4
                                    S.op("pe", lambda pe: pe.transpose(out=pt[:, c4 * P:(c4 + 1) * P],
                                                                       in_=hms[j][:, c * P:(c + 1) * P], identity=ident[:]),
                                         reads=[hbs[j], identb], writes=[pb])
                                src_ = pt[:, :].rearrange("p (c t) -> p c t", c=4)
                                if cb % 2 == 0:
                                    S.op("act", lambda a: a.activation(out=hT[j][:, cb * 4:cb * 4 + 4, :], in_=src_, func=AF.Copy),
                                         reads=[pb], writes=[hTb[j]])
                                else:
                                    S.op("dve", lambda v: v.tensor_copy(out=hT[j][:, cb * 4:cb * 4 + 4, :], in_=src_),
                                         reads=[pb], writes=[hTb[j]])

                        def sD(j=j, hold=hold):
                            pl, plb = PS.get()
                            for c in range(NCH):
                                S.op("pe", lambda pe: pe.matmul(pl[:, 0:NE], lhsT=hT[j][:, c, :], rhs=wr[:, c, :],
                                                                start=(c == 0), stop=(c == NCH - 1)),
                                     reads=[hTb[j], wrb], writes=[plb])
                            L = lg[j]
                            Lb = lgb[j]
                            S.op("dve", lambda v: v.tensor_reduce(out=L[:, 16:17], in_=pl[:, 0:NE], op=ALU.max, axis=AX.X),
                                 reads=[plb], writes=[Lb])
                            S.op("dve", lambda v: v.tensor_scalar(L[:, 17:18], L[:, 16:17], -1.0, None, op0=ALU.mult),
                                 reads=[Lb], writes=[Lb])
                            S.op("act", lambda a: a.activation(out=L[:, 0:NE], in_=pl[:, 0:NE], func=AF.Exp,
                                                               bias=L[:, 17:18], scale=1.0, accum_out=L[:, 18:19]),
                                 reads=[plb, Lb], writes=[Lb])
                            S.op("dve", lambda v: v.reciprocal(L[:, 19:20], L[:, 18:19]), reads=[Lb], writes=[Lb])
                            S.op("dve", lambda v: v.tensor_scalar(L[:, 32:32 + NE], L[:, 0:NE], L[:, 19:20], None,
                                                                  op0=ALU.mult), reads=[Lb], writes=[Lb])

                        def sE(j=j, dst=dst, dstb=dstb):
                            L = lg[j]
                            Lb = lgb[j]
                            pa, pab = PS.get()
                            S.op("pe", lambda pe: pe.transpose(out=pa[0:NE, 0:P], in_=L[:, 32:32 + NE], identity=ident[:]),
                                 reads=[Lb, identb], writes=[pab])
                            S.op("act", lambda a: a.activation(out=dst, in_=pa[0:NE, 0:P], func=AF.Copy),
                                 reads=[pab], writes=[dstb])
                        tiles_st.append([sA, sB, sC, sD, sE])
                    run_pipelined(tiles_st)
            vals = sb(nc, es2, "fb_vals", [48, CAPL], F32)
            idxu = sb(nc, es2, "fb_idxu", [48, CAPL], U32)
            idxf = sb(nc, es2, "fb_idxf", [48, CAPL], F32)
            valsc = sb(nc, es2, "fb_valsc", [48, CAPC], F32)
            idxuc = sb(nc, es2, "fb_idxuc", [48, CAPC], U32)
            idxfc = sb(nc, es2, "fb_idxfc", [48, CAPC], F32)
            tkb = Buf()

            def topk(work, workb, vv, iu, cap):
                for r in range(cap // 8):
                    sl = slice(r * 8, r * 8 + 8)
                    S.op("dve", lambda v: v.max(out=vv[:, sl], in_=work), reads=[workb], writes=[tkb])
                    S.op("dve", lambda v: v.max_index(out=iu[:, sl], in_max=vv[:, sl], in_values=work),
                         reads=[workb, tkb], writes=[tkb])
                    S.op("dve", lambda v: v.match_replace(out=work, in_to_replace=vv[:, sl], in_values=work,
                                                          imm_value=-1.0), reads=[tkb], writes=[workb])

            topk(affl[:, :], afflb, vals, idxu, CAPL)
            S.op("dve", lambda v: v.tensor_copy(out=idxf[:, :], in_=idxu[:, :]), reads=[tkb], writes=[tkb])
            for s in range(NS):
                S.op("dve", lambda v: v.tensor_scalar(idxf[s * 32:s * 32 + NE, :], idxf[s * 32:s * 32 + NE, :],
                                                      float(s * TS + TC), None, op0=ALU.add), reads=[tkb], writes=[tkb])
            if do_ctx:
                topk(affc[:, :], affcb, valsc, idxuc, CAPC)
                S.op("dve", lambda v: v.tensor_copy(out=idxfc[:, :], in_=idxuc[:, :]), reads=[tkb], writes=[tkb])
                S.op("dve", lambda v: v.tensor_scalar(idxfc[32:32 + NE, :], idxfc[32:32 + NE, :],
                                                      float(TS), None, op0=ALU.add), reads=[tkb], writes=[tkb])
            for c in range(2):
                for (src_, dst) in ((idxf, idxl), (vals, gatel)):
                    pt, pb = PS.get()
                    S.op("pe", lambda pe: pe.transpose(out=pt[:, 0:48], in_=src_[:, c * P:(c + 1) * P],
                                                       identity=ident[0:48, 0:48]),
                         reads=[tkb, identb], writes=[pb])
                    for s in range(NS):
                        S.op("dve", lambda v: v.tensor_copy(out=dst[:, s * 2 + c, :], in_=pt[:, s * 32:s * 32 + NE]), reads=[pb], writes=[tabb])
            if do_ctx:
                for (src_, dst) in ((idxfc, idxc), (valsc, gatec)):
                    pt, pb = PS.get()
                    S.op("pe", lambda pe: pe.transpose(out=pt[0:CAPC, 0:48], in_=src_[:, :], identity=ident[0:48, 0:48]),
                         reads=[tkb, identb], writes=[pb])
                    S.op("dve", lambda v: v.tensor_copy(out=dst[0:CAPC, :], in_=pt[0:CAPC, 0:NE]), reads=[pb], writes=[tabb])
                    S.op("dve", lambda v: v.tensor_copy(out=dst[CAPC:2 * CAPC, :], in_=pt[0:CAPC, 32:32 + NE]), reads=[pb], writes=[tabb])
        S.barrier()
        with contextlib.ExitStack() as es3:
            NWB = 6
            wbuf = [sb(nc, es3, "fc_w%d" % i, [P, 16 * 512], BF16) for i in range(NWB)]
            wbb = [Buf() for _ in range(NWB)]
            wi = [0]
            xg = [sb(nc, es3, "fc_xg%d" % i, [P, D], F32) for i in range(2)]
            xgb = [Buf(), Buf()]
            xsT = [sb(nc, es3, "fc_xsT%d" % i, [P, NCH, 576], BF16) for i in range(2)]
            xsTb = [Buf(), Buf()]
            hidT = sb(nc, es3, "fc_hidT", [P, 8, 576], BF16)
            hidTb = Buf()
            sg = [sb(nc, es3, "fc_sg%d" % i, [P, 576], F32) for i in range(2)]
            sgb = [Buf(), Buf()]
            ysb = [sb(nc, es3, "fc_y%d" % i, [P, D], F32) for i in range(2)]
            ysbb = [Buf(), Buf()]
            m5 = [sb(nc, es3, "fc_m5_%d" % i, [P, D], F32) for i in range(3)]
            m5b = Buf()
            for ms in range(3):
                S.dma("sp", lambda q: q.dma_start(out=m5[ms][:], in_=MODR[l, ms, 5:6, :].partition_broadcast(P)),
                      writes=[m5b])
            xrow = [Buf(), Buf(), Buf()]
            ncols = 576 if do_ctx else 512
            halves = ((0, 512), (512, 576)) if do_ctx else ((0, 512),)

            def wload(src_ap, shape3):
                i = wi[0] % NWB
                wi[0] += 1
                a, b = shape3
                view = wbuf[i][:, 0:a * b].rearrange("p (a b) -> p a b", a=a)
                S.dma("pool", lambda q: q.dma_start(out=view, in_=src_ap), writes=[wbb[i]])
                return view, wbb[i]

            tiles = [(rt, P, rt * P) for rt in range(4)] + ([(4, NS * CAPC, 512)] if do_ctx else [])
            cnt = {"gi": 0, "yi": 0}

            def stage_gather(e):
                xs_, xsb_ = xsT[e % 2], xsTb[e % 2]
                for (rt, rows, c0) in tiles:
                    j = cnt["gi"] % 2
                    cnt["gi"] += 1
                    idx_ap = idxl[:, rt, e:e + 1] if rt < 4 else idxc[0:rows, e:e + 1]
                    S.dma("pool", lambda q: q.indirect_dma_start(
                        out=xg[j][0:rows, :], out_offset=None, in_=H2[:, :],
                        in_offset=bass.IndirectOffsetOnAxis(ap=idx_ap, axis=0)),
                        reads=[tabb], writes=[xgb[j]])
                    for cb in range(4):
                        pt, pb = PS.get()
                        for c4 in range(4):
                            c = cb * 4 + c4
                            S.op("pe", lambda pe: pe.transpose(out=pt[:, c4 * P:c4 * P + rows],
                                                               in_=xg[j][0:rows, c * P:(c + 1) * P],
                                                               identity=ident[0:rows, 0:rows]),
                                 reads=[xgb[j], identb], writes=[pb])
                        src_ = pt[:, :].rearrange("p (c t) -> p c t", c=4)[:, :, 0:rows]
                        dst = xs_[:, cb * 4:cb * 4 + 4, c0:c0 + rows]
                        if cb % 2 == 0:
                            S.op("act", lambda a: a.activation(out=dst, in_=src_, func=AF.Copy), reads=[pb], writes=[xsb_])
                        else:
                            S.op("dve", lambda v: v.tensor_copy(out=dst, in_=src_), reads=[pb], writes=[xsb_])

            def stage_gateup(e):
                xs_, xsb_ = xsT[e % 2], xsTb[e % 2]
                for fb in range(2):
                    wg, wgb = wload(k.exp_w_gate[l, e, :, fb * 512:(fb + 1) * 512].rearrange("(c p) f -> p c f", p=P), (16, 512))
                    wu, wub = wload(k.exp_w_up[l, e, :, fb * 512:(fb + 1) * 512].rearrange("(c p) f -> p c f", p=P), (16, 512))
                    for f4 in range(4):
                        fc = fb * 4 + f4
                        pgs = [PS.get() for _ in halves]
                        pus = [PS.get() for _ in halves]
                        for (w_, wb_, pp) in ((wg, wgb, pgs), (wu, wub, pus)):
                            for c in range(NCH):
                                for hi, (h0, h1) in enumerate(halves):
                                    S.op("pe", lambda pe: pe.matmul(pp[hi][0][:, 0:h1 - h0], lhsT=w_[:, c, f4 * P:(f4 + 1) * P],
                                                                    rhs=xs_[:, c, h0:h1], start=(c == 0), stop=(c == NCH - 1)),
                                         reads=[wb_, xsb_], writes=[pp[hi][1]])
                        for hi, (h0, h1) in enumerate(halves):
                            pg, pgb = pgs[hi]
                            pu, pub = pus[hi]
                            j = (fc + (h0 > 0)) % 2
                            S.op("act", lambda a: a.activation(out=sg[j][:, 0:h1 - h0], in_=pg[:, 0:h1 - h0], func=AF.Silu),
                                 reads=[pgb], writes=[sgb[j]])
                            S.op("dve", lambda v: v.tensor_tensor(out=hidT[:, fc, h0:h1], in0=sg[j][:, 0:h1 - h0],
                                                                  in1=pu[:, 0:h1 - h0], op=ALU.mult),
                                 reads=[sgb[j], pub], writes=[hidTb])

            def stage_down(e):
                wds = []
                for hb in range(2):
                    wds.append(wload(k.exp_w_down[l, e, hb * 512:(hb + 1) * 512, :].rearrange("(c p) d -> p c d", p=P), (4, D)))
                for (rt, rows, c0) in tiles:
                    j = cnt["yi"] % 2
                    cnt["yi"] += 1
                    gate_ap = gatel[:, rt, e:e + 1] if rt < 4 else gatec[0:rows, e:e + 1]
                    mrow = m5[rt // 2] if rt < 4 else m5[2]
                    pys = [PS.get() for _ in range(4)]
                    for fc in range(8):
                        wd, wdb = wds[fc // 4]
                        for cb in range(4):
                            S.op("pe", lambda pe: pe.matmul(pys[cb][0][0:rows, :], lhsT=hidT[:, fc, c0:c0 + rows],
                                                            rhs=wd[:, fc % 4, cb * 512:(cb + 1) * 512],
                                                            start=(fc == 0), stop=(fc == 7)),
                                 reads=[hidTb, wdb], writes=[pys[cb][1]])
                    for cb in range(4):
                        py, pyb = pys[cb]
                        S.op("dve", lambda v: v.scalar_tensor_tensor(out=ysb[j][0:rows, cb * 512:(cb + 1) * 512],
                                                                     in0=py[0:rows, :], scalar=gate_ap,
                                                                     in1=mrow[0:rows, cb * 512:(cb + 1) * 512],
                                                                     op0=ALU.mult, op1=ALU.mult),
                             reads=[pyb, tabb, m5b], writes=[ysbb[j]])
                    idx_ap = idxl[:, rt, e:e + 1] if rt < 4 else idxc[0:rows, e:e + 1]
                    xb_ = xrow[rt // 2] if rt < 4 else xrow[2]
                    S.dma("pool", lambda q: q.indirect_dma_start(
                        out=X[:, :], out_offset=bass.IndirectOffsetOnAxis(ap=idx_ap, axis=0),
                        in_=ysb[j][0:rows, :], in_offset=None, compute_op=ALU.add),
                        reads=[tabb, ysbb[j]], writes=[xb_])

            for e in range(NE):
                stage_gather(e)
                stage_gateup(e)
                stage_down(e)
        S.barrier()


def mod_phase(k, S, layers=range(DEPTH)):
    nc = k.nc
    with contextlib.ExitStack() as es:
        PS = PsumPool(nc, es)
        ident = sb(nc, es, "m_ident", [P, P], F32)
        identb = Buf()
        S.dma("sp", lambda q: q.dma_start(out=ident[:], in_=k.ident_d[:, :]), writes=[identb])
        craw = sb(nc, es, "m_craw", [4, D], F32)
        cact = sb(nc, es, "m_cact", [4, D], F32)
        cb_ = Buf()
        S.op("dve", lambda v: v.memset(craw[:], 0.0), writes=[cb_])
        S.dma("sp", lambda q: q.dma_start(out=craw[0:2, :], in_=k.c[:, :]), writes=[cb_])
        S.dma("sp", lambda q: q.dma_start(out=craw[2:3, :], in_=k.c_ctx[:, :]), writes=[cb_])
        S.op("act", lambda a: a.activation(out=cact[:], in_=craw[:], func=AF.Silu), reads=[cb_], writes=[cb_])
        cT = sb(nc, es, "m_cT", [P, NCH, 4], BF16)
        cTb = Buf()
        for c in range(NCH):
            pt, pb = PS.get()
            S.op("pe", lambda pe: pe.transpose(out=pt[:, 0:4], in_=cact[:, c * P:(c + 1) * P], identity=ident[0:4, 0:4]),
                 reads=[cb_, identb], writes=[pb])
            S.op("dve", lambda v: v.tensor_copy(out=cT[:, c, :], in_=pt[:, 0:4]), reads=[pb], writes=[cTb])
        wts = [sb(nc, es, "m_w%d" % i, [P, NCH, 512], BF16) for i in range(4)]
        wtb = [Buf() for _ in range(4)]
        bm = [sb(nc, es, "m_b%d" % i, [4, 512], F32) for i in range(2)]
        bmb = [Buf(), Buf()]
        ot = [sb(nc, es, "m_o%d" % i, [4, 512], F32) for i in range(2)]
        otb = [Buf(), Buf()]
        it = 0
        for l in layers:
            for cb in range(24):
                j = it % 4
                j2 = it % 2
                it += 1
                v_, c0 = cb // 4, (cb % 4) * 512
                S.dma("pool", lambda q: q.dma_start(
                    out=wts[j][:, :, :],
                    in_=k.w_mod[l, :, cb * 512:(cb + 1) * 512].rearrange("(c p) n -> p c n", p=P)),
                    writes=[wtb[j]])
                S.dma("sp", lambda q: q.dma_start(out=bm[j2][:], in_=k.b_mod[l:l + 1, cb * 512:(cb + 1) * 512].partition_broadcast(4)),
                      writes=[bmb[j2]])
                pm, pmb = PS.get()
                for c in range(NCH):
                    S.op("pe", lambda pe: pe.matmul(pm[0:4, :], lhsT=cT[:, c, :], rhs=wts[j][:, c, :],
                                                    start=(c == 0), stop=(c == NCH - 1)),
                         reads=[cTb, wtb[j]], writes=[pmb])
                S.op("dve", lambda v: v.tensor_tensor(out=ot[j2][:], in0=pm[0:4, :], in1=bm[j2][:], op=ALU.add),
                     reads=[pmb, bmb[j2]], writes=[otb[j2]])
                S.dma("sp", lambda q: q.dma_start(out=k.MODR[l, 0:3, v_, c0:c0 + 512], in_=ot[j2][0:3, :]), reads=[otb[j2]])
    S.barrier()


def norm1_phase(k, S, l, s, HT, HTb, PS, ident, identb, do_ctx=True):
    nc = k.nc
    NBN = 3
    with contextlib.ExitStack() as es2:
        xts = [sb(nc, es2, "n1_xt%d" % i, [P, D], F32) for i in range(NBN)]
        xbs = [Buf() for _ in range(NBN)]
        hms = [sb(nc, es2, "n1_hm%d" % i, [P, D], F32) for i in range(NBN)]
        hbs = [Buf() for _ in range(NBN)]
        smalls = [sb(nc, es2, "n1_sm%d" % i, [P, 8], F32) for i in range(NBN)]
        smbs = [Buf() for _ in range(NBN)]
        grow = sb(nc, es2, "n1_grow", [P, D], F32)
        srow = sb(nc, es2, "n1_srow", [P, D], F32)
        gnorm = sb(nc, es2, "n1_gnorm", [P, D], F32)
        gsb = Buf()
        gnb = Buf()
        S.dma("sp", lambda q: q.dma_start(out=gnorm[:], in_=k.norm_mix_g[l:l + 1, :].partition_broadcast(P)), writes=[gnb])
        it = 0
        for grp in ((0, 1) if do_ctx else (1,)):
            ms = 2 if grp == 0 else s
            S.dma("sp", lambda q: q.dma_start(out=grow[:], in_=k.MODR[l, ms, 1:2, :].partition_broadcast(P)), writes=[gsb])
            S.dma("sp", lambda q: q.dma_start(out=srow[:], in_=k.MODR[l, ms, 0:1, :].partition_broadcast(P)), writes=[gsb])
            S.op("dve", lambda v: v.scalar_tensor_tensor(out=grow[:], in0=grow[:], scalar=1.0, in1=gnorm[:],
                                                         op0=ALU.add, op1=ALU.mult), reads=[gnb], writes=[gsb])
            ntile = (TC if grp == 0 else TL) // P
            rbase = s * TS + (0 if grp == 0 else TC)
            tiles_st = []
            for t in range(ntile):
                j = it % NBN
                it += 1
                r0 = rbase + t * P
                col0 = (0 if grp == 0 else TC) + t * P
                sA, sB = norm_stages(S, k.X, r0, xts[j], xbs[j], hms[j], hbs[j], smalls[j], smbs[j], grow, srow, gsb)

                def sC(j=j, col0=col0):
                    for cb in range(4):
                        pt, pb = PS.get()
                        for c4 in range(4):
                            c = cb * 4 + c4
                            S.op("pe", lambda pe: pe.transpose(out=pt[:, c4 * P:(c4 + 1) * P], in_=hms[j][:, c * P:(c + 1) * P],
                                                               identity=ident[:]), reads=[hbs[j], identb], writes=[pb])
                        src_ = pt[:, :].rearrange("p (c t) -> p c t", c=4)
                        dst = HT[:, cb * 4:cb * 4 + 4, col0:col0 + P]
                        if cb % 2 == 0:
                            S.op("act", lambda a: a.activation(out=dst, in_=src_, func=AF.Copy), reads=[pb], writes=[HTb])
                        else:
                            S.op("dve", lambda v: v.tensor_copy(out=dst, in_=src_), reads=[pb], writes=[HTb])
                tiles_st.append([sA, sB, sC])
            run_pipelined(tiles_st)


def outproj_phase(k, S, l, s, do_ctx=True):
    nc = k.nc
    with contextlib.ExitStack() as es:
        PS = PsumPool(nc, es)
        YT = sb(nc, es, "op_YT", [P, NCH, TS], BF16)
        YTb = Buf()
        for c in range(NCH):
            S.dma("sp", lambda q: q.dma_start(out=YT[:, c, :], in_=k.YTd[c, :, :]), writes=[YTb])
        m2 = [sb(nc, es, "op_m2_%d" % i, [P, D], F32) for i in range(2)]
        m2b = Buf()
        S.dma("sp", lambda q: q.dma_start(out=m2[0][:], in_=k.MODR[l, 2, 2:3, :].partition_broadcast(P)), writes=[m2b])
        S.dma("sp", lambda q: q.dma_start(out=m2[1][:], in_=k.MODR[l, s, 2:3, :].partition_broadcast(P)), writes=[m2b])
        wts = [sb(nc, es, "op_w%d" % i, [P, NCH, 512], BF16) for i in range(2)]
        wtb = [Buf(), Buf()]
        xt = [sb(nc, es, "op_x%d" % i, [P, 512], F32) for i in range(6)]
        xtb = [Buf() for _ in range(6)]
        it = 0
        for cb in range(4):
            j = cb % 2
            S.dma("pool", lambda q: q.dma_start(out=wts[j][:], in_=k.w_out[l, :, cb * 512:(cb + 1) * 512].rearrange("(c p) n -> p c n", p=P)),
                  writes=[wtb[j]])
            for t in range(0 if do_ctx else 2, TS // P):
                i = it % 6
                it += 1
                r0 = s * TS + t * P
                S.dma("sp", lambda q: q.dma_start(out=xt[i][:], in_=k.X[r0:r0 + P, cb * 512:(cb + 1) * 512]), writes=[xtb[i]])
                py, pyb = PS.get()
                for c in range(NCH):
                    S.op("pe", lambda pe: pe.matmul(py[:, :], lhsT=YT[:, c, t * P:(t + 1) * P], rhs=wts[j][:, c, :],
                                                    start=(c == 0), stop=(c == NCH - 1)), reads=[YTb, wtb[j]], writes=[pyb])
                mrow = m2[0] if t < 2 else m2[1]
                tmp = py
                S.op("dve", lambda v: v.tensor_tensor(out=py[:, :], in0=py[:, :], in1=mrow[:, cb * 512:(cb + 1) * 512], op=ALU.mult),
                     reads=[m2b], writes=[pyb])
                S.op("dve", lambda v: v.tensor_tensor(out=xt[i][:], in0=py[:, :], in1=xt[i][:], op=ALU.add),
                     reads=[pyb], writes=[xtb[i]])
                S.dma("act", lambda q: q.dma_start(out=k.X[r0:r0 + P, cb * 512:(cb + 1) * 512], in_=xt[i][:]), reads=[xtb[i]])
    S.barrier()


def na_band_start(i):
    return min(max(2 * i - 4, 0), 22)


def na_tab_type(i):
    return {0: 0, 1: 1, 14: 3, 15: 4}.get(i, 2)


def na_phase(k, S, l, s, do_ctx=True):
    nc = k.nc
    ia = l // 2
    scale = 128 ** -0.5
    NB = 4
    with contextlib.ExitStack() as es:
        PS = PsumPool(nc, es, n=6)
        PSB = PsumPool(nc, es, n=2, dt=BF16, width=1024)
        ident = sb(nc, es, "na_ident", [P, P], F32)
        identb = Buf()
        S.dma("sp", lambda q: q.dma_start(out=ident[:], in_=k.ident_d[:, :]), writes=[identb])
        ident16 = sb(nc, es, "na_ident16", [P, P], BF16)
        S.op("dve", lambda v: v.tensor_copy(out=ident16[:], in_=ident[:]), reads=[identb], writes=[identb])
        HT = sb(nc, es, "na_HT", [P, NCH, TS], BF16)
        HTb = Buf()
        norm1_phase(k, S, l, s, HT, HTb, PS, ident, identb, do_ctx=True)
        S.barrier()
        wq = [sb(nc, es, "na_wq%d" % i, [P, NCH, 2, P], BF16) for i in range(2)]
        wqb = [Buf(), Buf()]
        tab = [sb(nc, es, "na_tab%d" % i, [P, 5, 640], F32) for i in range(2)]
        tabb = [Buf(), Buf()]
        QT = sb(nc, es, "na_QT", [P, TS], BF16)
        KT = sb(nc, es, "na_KT", [P, TS], BF16)
        V4 = sb(nc, es, "na_V4", [P, TS // P, 4 * P], BF16)
        V4b = Buf()
        wv4 = sb(nc, es, "na_wv4", [P, NCH, 4 * P], BF16)
        wv4b = Buf()
        qkvb = Buf()
        YTh = [sb(nc, es, "na_YTh%d" % i, [P, TS], BF16) for i in range(2)]
        YThb = [Buf(), Buf()]
        NB1, NB2, NB3 = 6, 6, 5
        sc = [sb(nc, es, "na_sc%d" % i, [P, 896], F32) for i in range(NB1)]
        scb = [[Buf(), Buf(), Buf()] for _ in range(NB1)]
        pbf = [sb(nc, es, "na_pbf%d" % i, [P, 896], BF16) for i in range(NB2)]
        pbfb = [Buf() for _ in range(NB2)]
        st = [sb(nc, es, "na_st%d" % i, [P, 8], F32) for i in range(NB1)]
        stb = [Buf() for _ in range(NB1)]
        PT = [sb(nc, es, "na_PT%d" % i, [P, 7, P], BF16) for i in range(NB3)]
        PTb = [Buf() for _ in range(NB3)]
        if not do_ctx:
            for i in range(2):
                S.op("dve", lambda v: v.memset(YTh[i][:, 0:TC], 0.0), writes=[YThb[i]])
        it = 0
        for h in range(16):
            j = h % 2
            if h % 4 == 0:
                S.dma("pool", lambda q: q.dma_start(
                    out=wv4[:, :, :],
                    in_=k.na_w_qkv[ia, :, 2 * D + h * P:2 * D + (h + 4) * P].rearrange("(c p) n -> p c n", p=P)), writes=[wv4b])
                for t in range(TS // P):
                    pv, pvb = PS.get()
                    for c in range(NCH):
                        S.op("pe", lambda pe: pe.matmul(pv[:, :], lhsT=HT[:, c, t * P:(t + 1) * P], rhs=wv4[:, c, :],
                                                        start=(c == 0), stop=(c == NCH - 1)), reads=[wv4b, HTb], writes=[pvb])
                    if t % 2 == 0:
                        S.op("act", lambda a: a.activation(out=V4[:, t, :], in_=pv[:, :], func=AF.Copy), reads=[pvb], writes=[V4b])
                    else:
                        S.op("dve", lambda v: v.tensor_copy(out=V4[:, t, :], in_=pv[:, :]), reads=[pvb], writes=[V4b])
            for qi in range(2):
                S.dma("pool", lambda q: q.dma_start(
                    out=wq[j][:, :, qi, :],
                    in_=k.na_w_qkv[ia, :, qi * D + h * P:qi * D + (h + 1) * P].rearrange("(c p) n -> p c n", p=P)),
                    writes=[wqb[j]])
            S.dma("sp", lambda q: q.dma_start(out=tab[j][:], in_=k.na_tab[ia, h].rearrange("t p n -> p t n")), writes=[tabb[j]])
            for qi, dstT in ((0, QT), (1, KT)):
                for tb in range(5):
                    n0 = tb * 512
                    n1 = min(TS, n0 + 512)
                    pq, pqb = PS.get()
                    for c in range(NCH):
                        S.op("pe", lambda pe: pe.matmul(pq[:, 0:n1 - n0], lhsT=wq[j][:, c, qi, :], rhs=HT[:, c, n0:n1],
                                                        start=(c == 0), stop=(c == NCH - 1)), reads=[wqb[j], HTb], writes=[pqb])
                    sc_ = scale if qi == 0 else 1.0
                    if tb % 2 == 0:
                        S.op("act", lambda a: a.activation(out=dstT[:, n0:n1], in_=pq[:, 0:n1 - n0], func=AF.Copy, scale=sc_),
                             reads=[pqb], writes=[qkvb])
                    else:
                        S.op("dve", lambda v: v.tensor_scalar(dstT[:, n0:n1], pq[:, 0:n1 - n0], sc_, None, op0=ALU.mult),
                             reads=[pqb], writes=[qkvb])
            qtiles = ([("c", 0), ("c", 1)] if do_ctx else []) + [("l", i) for i in range(16)]

            def make_tile(kind, i, tix, j=j, hq=h % 4):
                u1, u2, u3 = tix % NB1, tix % NB2, tix % NB3
                par = tix % 2
                hold = {}
                if kind == "c":
                    q0, nk, vtiles = i * P, 256, [0, 1]
                else:
                    q0, nk = TC + i * P, 896
                    bs = na_band_start(i)
                    k0 = TC + bs * 64
                    tt = na_tab_type(i)
                    vtiles = [2 + bs // 2 + c for c in range(5)] + [0, 1]
                nchunk = nk // P

                def s1():
                    pa, pab = PS.get()
                    hold["pa"] = (pa, pab)
                    if kind == "c":
                        S.op("pe", lambda pe: pe.matmul(pa[:, 0:256], lhsT=QT[:, q0:q0 + P], rhs=KT[:, 0:256], start=True, stop=True),
                             reads=[qkvb], writes=[pab])
                    else:
                        pb2, pb2b = PS.get()
                        hold["pb2"] = (pb2, pb2b)
                        S.op("pe", lambda pe: pe.matmul(pa[:, 0:512], lhsT=QT[:, q0:q0 + P], rhs=KT[:, k0:k0 + 512], start=True, stop=True),
                             reads=[qkvb], writes=[pab])
                        S.op("pe", lambda pe: pe.matmul(pb2[:, 0:128], lhsT=QT[:, q0:q0 + P], rhs=KT[:, k0 + 512:k0 + 640], start=True, stop=True),
                             reads=[qkvb], writes=[pb2b])
                        S.op("pe", lambda pe: pe.matmul(pb2[:, 128:384], lhsT=QT[:, q0:q0 + P], rhs=KT[:, 0:256], start=True, stop=True),
                             reads=[qkvb], writes=[pb2b])

                def s2():
                    pa, pab = hold["pa"]
                    if kind == "c":
                        S.op("act", lambda a: a.activation(out=sc[u1][:, 0:256], in_=pa[:, 0:256], func=AF.Copy),
                             reads=[pab], writes=[scb[u1][0]])
                    else:
                        pb2, pb2b = hold["pb2"]
                        S.op("dve", lambda v: v.tensor_tensor(out=sc[u1][:, 0:512], in0=pa[:, 0:512], in1=tab[j][:, tt, 0:512], op=ALU.add),
                             reads=[pab, tabb[j]], writes=[scb[u1][0]])
                        S.op("dve", lambda v: v.tensor_tensor(out=sc[u1][:, 512:640], in0=pb2[:, 0:128], in1=tab[j][:, tt, 512:640], op=ALU.add),
                             reads=[pb2b, tabb[j]], writes=[scb[u1][1]])
                        S.op("act", lambda a: a.activation(out=sc[u1][:, 640:896], in_=pb2[:, 128:384], func=AF.Copy),
                             reads=[pb2b], writes=[scb[u1][2]])

                def s3():
                    S.op("dve", lambda v: v.tensor_reduce(out=st[u1][:, 1:2], in_=sc[u1][:, 0:nk], op=ALU.max, axis=AX.X),
                         reads=scb[u1], writes=[stb[u1]])
                    S.op("dve", lambda v: v.tensor_scalar(st[u1][:, 0:1], st[u1][:, 1:2], -1.0, None, op0=ALU.mult),
                         reads=[stb[u1]], writes=[stb[u1]])

                def s4():
                    S.op("act", lambda a: a.activation(out=sc[u1][:, 0:nk], in_=sc[u1][:, 0:nk], func=AF.Exp, bias=st[u1][:, 0:1], scale=1.0,
                                                       accum_out=st[u1][:, 2:3]), reads=[stb[u1]], writes=scb[u1] + [stb[u1]])

                def s5():
                    S.op("dve", lambda v: v.reciprocal(st[u1][:, 3:4], st[u1][:, 2:3]), reads=[stb[u1]], writes=[stb[u1]])
                    if par == 0:
                        S.op("act", lambda a: a.activation(out=pbf[u2][:, 0:nk], in_=sc[u1][:, 0:nk], func=AF.Copy, scale=st[u1][:, 3:4]),
                             reads=[stb[u1]] + scb[u1], writes=[pbfb[u2]])
                    else:
                        S.op("dve", lambda v: v.tensor_scalar(pbf[u2][:, 0:nk], sc[u1][:, 0:nk], st[u1][:, 3:4], None, op0=ALU.mult),
                             reads=[stb[u1]] + scb[u1], writes=[pbfb[u2]])

                def s6():
                    pt, ptb = PSB.get()
                    hold["pt"] = (pt, ptb)
                    for c in range(nchunk):
                        S.op("pe", lambda pe: pe.transpose(out=pt[:, c * P:(c + 1) * P], in_=pbf[u2][:, c * P:(c + 1) * P], identity=ident16[:]),
                             reads=[pbfb[u2], identb], writes=[ptb])

                def s7():
                    pt, ptb = hold["pt"]
                    S.op("act", lambda a: a.activation(out=PT[u3][:, 0:nchunk, :], in_=pt[:, 0:nk].rearrange("p (c t) -> p c t", c=nchunk), func=AF.Copy),
                         reads=[ptb], writes=[PTb[u3]])

                def s8():
                    po, pob = PS.get()
                    hold["po"] = (po, pob)
                    for c in range(nchunk):
                        S.op("pe", lambda pe: pe.matmul(po[:, 0:P], lhsT=V4[:, vtiles[c], hq * P:(hq + 1) * P], rhs=PT[u3][:, c, :],
                                                        start=(c == 0), stop=(c == nchunk - 1)), reads=[V4b, PTb[u3]], writes=[pob])

                def s9():
                    po, pob = hold["po"]
                    S.op("dve", lambda v: v.tensor_copy(out=YTh[j][:, q0:q0 + P], in_=po[:, 0:P]), reads=[pob], writes=[YThb[j]])
                def grp(*fs):
                    def run():
                        for f_ in fs:
                            f_()
                    return run
                if NA_STAGES == 4:
                    return [grp(s1, s2, s3), grp(s4, s5), grp(s6, s7), grp(s8, s9)]
                return [s1, s2, s3, s4, s5, s6, s7, s8, s9]

            tiles_st = []
            for (kind, i) in qtiles:
                tiles_st.append(make_tile(kind, i, it))
                it += 1
            if NA_PAIR > 1:
                merged = []
                for p0 in range(0, len(tiles_st), NA_PAIR):
                    grp_ = tiles_st[p0:p0 + NA_PAIR]

                    def mk(si, grp_=grp_):
                        def run():
                            for ts_ in grp_:
                                ts_[si]()
                        return run
                    merged.append([mk(si) for si in range(len(grp_[0]))])
                tiles_st = merged
            run_pipelined(tiles_st)
            if NA_HEAD_BARRIER:
                S.barrier()
            S.dma("sp", lambda q: q.dma_start(out=k.YTd[h, :, :], in_=YTh[j][:, :]), reads=[YThb[j]])
    S.barrier()


def build_na_tab(rpb):
    A = rpb.shape[0]
    tab = np.full((A, 16, 5, 128, 640), -30000.0, np.float32)
    for tt, i in enumerate([0, 1, 2, 14, 15]):
        bs = na_band_start(i)
        for rr in range(2):
            r = 2 * i + rr
            rs = min(max(r - 4, 0), 24)
            for c in range(64):
                cs = min(max(c - 8, 0), 48)
                q = rr * 64 + c
                kc = np.arange(cs, cs + 16)
                for j in range(10):
                    kr = bs + j
                    if not (rs <= kr < rs + 8):
                        continue
                    tab[:, :, tt, q, j * 64 + kc] = rpb[:, :, kr - r + 7, kc - c + 15]
    return tab


REC_BLOCKS = [(0, 256), (256, 768), (768, 1280), (1280, 1792), (1792, 2304)]
C_Q, C_K, C_V, C_G, C_ZA, C_LX, C_LG = 0, 512, 1024, 2048, 3072, 3104, 4128


def build_rope_tabs():
    t = np.arange(TL)
    rows = (t // 64).astype(np.float32)
    cols = (t % 64).astype(np.float32)
    inv = (np.float32(10000.0) ** (-np.arange(32, dtype=np.float32) / np.float32(32))).astype(np.float32)
    ang = np.concatenate([rows[:, None] * inv, cols[:, None] * inv], axis=-1).astype(np.float32)
    cosT = np.cos(ang).astype(np.float32).T
    sinT = np.sin(ang).astype(np.float32).T
    cosF = np.repeat(cosT, 2, axis=0)
    sinF = np.repeat(sinT, 2, axis=0).copy()
    sinF[0::2] *= -1.0
    return np.ascontiguousarray(cosF), np.ascontiguousarray(sinF)


def proj_fm(S, PS, HT, HTb, wt, wtb, wsl, evac, blocks=REC_BLOCKS, m=P):
    for bi, (n0, n1) in enumerate(blocks):
        pq, pqb = PS.get()
        for c in range(NCH):
            S.op("pe", lambda pe: pe.matmul(pq[0:m, 0:n1 - n0], lhsT=wsl(c), rhs=HT[:, c, n0:n1],
                                            start=(c == 0), stop=(c == NCH - 1)), reads=[wtb, HTb], writes=[pqb])
        evac(pq, pqb, n0, n1, bi)


def rec_phase(k, S, l, s):
    nc = k.nc
    ir = l // 2
    NCK = TS // P
    with contextlib.ExitStack() as es:
        PS = PsumPool(nc, es, n=5)
        PSK = PsumPool(nc, es, n=3)
        ident = sb(nc, es, "r_ident", [P, P], F32)
        identb = Buf()
        S.dma("sp", lambda q: q.dma_start(out=ident[:], in_=k.ident_d[:, :]), writes=[identb])
        HT = sb(nc, es, "r_HT", [P, NCH, TS], BF16)
        HTb = Buf()
        norm1_phase(k, S, l, s, HT, HTb, PS, ident, identb, do_ctx=True)
        S.barrier()
        cst = Buf()
        with contextlib.ExitStack() as es2:
            maskf = sb(nc, es2, "g_maskf", [P, P], F32)
            maskb = sb(nc, es2, "g_maskb", [P, P], F32)
            ones = sb(nc, es2, "g_ones", [P, P], F32)
            S.dma("sp", lambda q: q.dma_start(out=maskf[:], in_=k.mask_f[:, :]), writes=[cst])
            S.dma("sp", lambda q: q.dma_start(out=maskb[:], in_=k.mask_b[:, :]), writes=[cst])
            S.op("dve", lambda v: v.memset(ones[:], 1.0), writes=[cst])
            cs_ = [sb(nc, es2, "g_cs%d" % i, [P, 2, 512], F32) for i in range(2)]
            csb = [Buf(), Buf()]
            aup = sb(nc, es2, "g_aup", [16, 2, 512], BF16)
            S.dma("pool", lambda q: q.dma_start(out=aup[:], in_=k.gla_alpha_up[ir].rearrange("d r n -> r d n")), writes=[cst])
            negab = sb(nc, es2, "g_negab", [P, 2, 4], F32)
            gg = sb(nc, es2, "g_gg", [P, 2], F32)
            with nc.allow_non_contiguous_dma(reason="tiny per-feature vectors"):
                S.dma("sp", lambda q: q.dma_start(out=negab[:], in_=k.gla_alpha_b[ir].rearrange("d (h p) -> p d h", p=P)), writes=[cst])
                S.dma("sp", lambda q: q.dma_start(out=gg[:], in_=k.gla_norm_g[ir].rearrange("(c p) -> p c", p=P)), writes=[cst])
            S.op("dve", lambda v: v.tensor_scalar(negab[:], negab[:], -1.0, None, op0=ALU.mult), writes=[cst])
            wza = sb(nc, es2, "g_wza", [P, NCH, 32], BF16)
            wzab = Buf()
            S.dma("pool", lambda q: q.dma_start(out=wza[:], in_=k.rec_w_in[ir, :, C_ZA:C_ZA + 32].rearrange("(c p) n -> p c n", p=P)),
                  writes=[wzab])
            zaT = [sb(nc, es2, "g_zaT%d" % d_, [16, TS], BF16) for d_ in range(2)]
            zab = Buf()
            for d_ in range(2):
                def ev_za(pq, pqb, n0, n1, bi, d_=d_):
                    S.op("act", lambda a: a.activation(out=zaT[d_][:, n0:n1], in_=pq[0:16, 0:n1 - n0], func=AF.Copy), reads=[pqb], writes=[zab])
                proj_fm(S, PS, HT, HTb, wza, wzab, lambda c, d_=d_: wza[:, c, d_ * 16:(d_ + 1) * 16], ev_za, m=16)
            wqk = sb(nc, es2, "g_wqk", [P, NCH, 4, P], BF16)
            wqkb = Buf()
            wvgb = wqkb
            q_r = sb(nc, es2, "g_qr", [P, TS], F32)
            k_r = sb(nc, es2, "g_kr", [P, TS], F32)
            qkb = Buf()
            sq = [sb(nc, es2, "g_sq%d" % i, [P, 2, 512], F32) for i in range(2)]
            sqb = [Buf(), Buf()]
            tmpb_ = [sq[i][:, 0, :] for i in range(2)]
            tmpbb = sqb
            Vh = sb(nc, es2, "g_V", [P, NCK, 256], BF16)
            Vb = Buf()
            Lc = sb(nc, es2, "g_Lc", [P, TS], F32)
            Lcb = Buf()
            Et = sb(nc, es2, "g_Et", [P, TS], F32)
            Etb = Buf()
            qd = sb(nc, es2, "g_qd", [P, TS], BF16)
            ki = sb(nc, es2, "g_ki", [P, TS], BF16)
            ke = Et
            dkb = Buf()
            cend = sb(nc, es2, "g_cend", [P, 2, NCK], F32)
            cendb = Buf()
            OT = sb(nc, es2, "g_OT", [P, 2, TS], F32)
            OTb = Buf()
            SG = [sb(nc, es2, "g_SG%d" % i, [P, 2, 512], BF16) for i in range(2)]
            SGb = [Buf(), Buf()]
            state = sb(nc, es2, "g_state", [P, 256], F32)
            stbf = sb(nc, es2, "g_stbf", [P, 256], BF16)
            stateb = Buf()
            stbfb = Buf()
            sTm = [sb(nc, es2, "g_sTm%d" % i, [P, P], BF16) for i in range(3)]
            sTmb = [Buf() for _ in range(3)]
            keT = [sb(nc, es2, "g_keT%d" % i, [P, P], BF16) for i in range(3)]
            keTb = [Buf() for _ in range(3)]
            rst = [sb(nc, es2, "g_rst%d" % i, [P, 512], F32) for i in range(2)]
            rstb = [Buf(), Buf()]
            YTc = [qd, ki]
            YTcb = [dkb, dkb]
            for hd in range(4):
                S.dma("pool", lambda q: q.dma_start(out=wqk[:, :, 0, :], in_=k.rec_w_in[ir, :, C_Q + hd * P:C_Q + (hd + 1) * P].rearrange("(c p) n -> p c n", p=P)), writes=[wqkb])
                S.dma("pool", lambda q: q.dma_start(out=wqk[:, :, 1, :], in_=k.rec_w_in[ir, :, C_K + hd * P:C_K + (hd + 1) * P].rearrange("(c p) n -> p c n", p=P)), writes=[wqkb])
                for a_ in range(2):
                    S.op("dve", lambda v: v.tensor_copy(out=wqk[:, :, 2 + a_, 0:P:2], in_=wqk[:, :, a_, 1:P:2]), reads=[wqkb], writes=[wqkb])
                    S.op("dve", lambda v: v.tensor_copy(out=wqk[:, :, 2 + a_, 1:P:2], in_=wqk[:, :, a_, 0:P:2]), reads=[wqkb], writes=[wqkb])
                for a_, dst in ((0, q_r), (1, k_r)):
                    for bi, (n0, n1) in enumerate(REC_BLOCKS):
                        pq, pqb = PS.get()
                        for c in range(NCH):
                            S.op("pe", lambda pe: pe.matmul(pq[:, 0:n1 - n0], lhsT=wqk[:, c, a_, :], rhs=HT[:, c, n0:n1],
                                                            start=(c == 0), stop=(c == NCH - 1)), reads=[wqkb, HTb], writes=[pqb])
                        if bi == 0:
                            S.op("act", lambda a: a.activation(out=dst[:, n0:n1], in_=pq[:, 0:n1 - n0], func=AF.Copy), reads=[pqb], writes=[qkb])
                            continue
                        ps_, psb_ = PS.get()
                        for c in range(NCH):
                            S.op("pe", lambda pe: pe.matmul(ps_[:, 0:n1 - n0], lhsT=wqk[:, c, 2 + a_, :], rhs=HT[:, c, n0:n1],
                                                            start=(c == 0), stop=(c == NCH - 1)), reads=[wqkb, HTb], writes=[psb_])
                        t0 = n0 - TC
                        u = bi % 2
                        S.dma("sp", lambda q: q.dma_start(out=cs_[u][:, 0, :], in_=k.rope_cos[:, t0:t0 + 512]), writes=[csb[u]])
                        S.dma("sp", lambda q: q.dma_start(out=cs_[u][:, 1, :], in_=k.rope_sin[:, t0:t0 + 512]), writes=[csb[u]])
                        S.op("dve", lambda v: v.tensor_tensor(out=tmpb_[u], in0=ps_[:, 0:512], in1=cs_[u][:, 1, :], op=ALU.mult),
                             reads=[psb_, csb[u]], writes=[tmpbb[u]])
                        S.op("dve", lambda v: v.tensor_tensor(out=dst[:, n0:n1], in0=pq[:, 0:512], in1=cs_[u][:, 0, :], op=ALU.mult),
                             reads=[pqb, csb[u]], writes=[qkb])
                        S.op("pool", lambda g: g.tensor_tensor(out=dst[:, n0:n1], in0=dst[:, n0:n1], in1=tmpb_[u], op=ALU.add),
                             reads=[tmpbb[u]], writes=[qkb])
                S.dma("pool", lambda q: q.dma_start(out=wqk[:, :, 0:2, :], in_=k.rec_w_in[ir, :, C_V + hd * 256:C_V + (hd + 1) * 256].rearrange("(c p) (a n) -> p c a n", p=P, a=2)), writes=[wvgb])
                S.dma("pool", lambda q: q.dma_start(out=wqk[:, :, 2:4, :], in_=k.rec_w_in[ir, :, C_G + hd * 256:C_G + (hd + 1) * 256].rearrange("(c p) (a n) -> p c a n", p=P, a=2)), writes=[wvgb])
                if hd == 0 and getattr(k, "dbg", None):
                    S.dma("sp", lambda q: q.dma_start(out=k.dbg["qr"][:, :], in_=q_r[:, :]), reads=[qkb])
                    S.dma("sp", lambda q: q.dma_start(out=k.dbg["kr"][:, :], in_=k_r[:, :]), reads=[qkb])
                for tg in range(0, NCK, 2):
                    pv, pvb = PS.get()
                    for t2 in range(2):
                        t = tg + t2
                        for c in range(NCH):
                            S.op("pe", lambda pe: pe.matmul(pv[:, t2 * 256:(t2 + 1) * 256], lhsT=HT[:, c, t * P:(t + 1) * P], rhs=wqk[:, c, 0:2, :],
                                                            start=(c == 0), stop=(c == NCH - 1)), reads=[wvgb, HTb], writes=[pvb])
                    S.op("act", lambda a: a.activation(out=Vh[:, tg:tg + 2, :], in_=pv[:, :].rearrange("p (t d) -> p t d", t=2), func=AF.Copy),
                         reads=[pvb], writes=[Vb])
                for dr in range(2):
                    for bi, (n0, n1) in enumerate(REC_BLOCKS):
                        pz, pzb = PS.get()
                        S.op("pe", lambda pe: pe.matmul(pz[:, 0:n1 - n0], lhsT=aup[:, dr, hd * P:(hd + 1) * P], rhs=zaT[dr][:, n0:n1],
                                                        start=True, stop=True), reads=[cst, zab], writes=[pzb])
                        S.op("act", lambda a: a.activation(out=Lc[:, n0:n1], in_=pz[:, 0:n1 - n0], func=AF.Exp, scale=-1.0,
                                                           bias=negab[:, dr, hd:hd + 1]), reads=[pzb, cst], writes=[Lcb])
                    S.op("act", lambda a: a.activation(out=Lc[:, :], in_=Lc[:, :], func=AF.Ln, bias=1.0, scale=1.0), reads=[Lcb], writes=[Lcb])
                    for n in range(NCK):
                        sl = slice(n * P, (n + 1) * P)
                        if dr == 0:
                            S.op("dve", lambda v: v.tensor_tensor_scan(out=Lc[:, sl], data0=ones[:, :], data1=Lc[:, sl], initial=0.0,
                                                                       op0=ALU.mult, op1=ALU.add), reads=[Lcb, cst], writes=[Lcb])
                        else:
                            rs_ = slice((n + 1) * P - 1, (n * P - 1) if n > 0 else None, -1)
                            S.op("dve", lambda v: v.tensor_tensor_scan(out=Lc[:, rs_], data0=ones[:, :], data1=Lc[:, rs_], initial=0.0,
                                                                       op0=ALU.mult, op1=ALU.add), reads=[Lcb, cst], writes=[Lcb])
                    if hd == 0 and getattr(k, "dbg", None):
                        S.dma("sp", lambda q: q.dma_start(out=k.dbg["lc%d" % dr][:, :], in_=Lc[:, :]), reads=[Lcb])
                    ce_src = Lc[:, P - 1:TS:P] if dr == 0 else Lc[:, 0:TS:P]
                    S.op("dve", lambda v: v.tensor_scalar(cend[:, 0, :], ce_src, -1.0 / 16.0, None, op0=ALU.mult), reads=[Lcb], writes=[cendb])
                    S.op("act", lambda a: a.activation(out=cend[:, 1, :], in_=cend[:, 0, :], func=AF.Exp), reads=[cendb], writes=[cendb])
                    S.op("act", lambda a: a.activation(out=Et[:, :], in_=Lc[:, :], func=AF.Exp, scale=-1.0 / 16.0), reads=[Lcb], writes=[Etb])
                    S.op("dve", lambda v: v.scalar_tensor_tensor(out=qd[:, :], in0=q_r[:, :], scalar=float(128 ** -0.5), in1=Et[:, :],
                                                                 op0=ALU.mult, op1=ALU.mult), reads=[qkb, Etb], writes=[dkb])
                    S.op("act", lambda a: a.activation(out=Et[:, :], in_=Lc[:, :], func=AF.Exp, scale=1.0 / 16.0), reads=[Lcb, dkb], writes=[Etb])
                    S.op("dve", lambda v: v.tensor_tensor(out=ki[:, :], in0=k_r[:, :], in1=Et[:, :], op=ALU.mult), reads=[qkb, Etb], writes=[dkb])
                    for n in range(NCK):
                        sl = slice(n * P, (n + 1) * P)
                        S.op("act", lambda a: a.activation(out=Et[:, sl], in_=Lc[:, sl], func=AF.Exp, scale=1.0 / 16.0, bias=cend[:, 0, n:n + 1]),
                             reads=[Lcb, cendb, dkb], writes=[Etb])
                    S.op("pool", lambda g: g.tensor_tensor(out=ke[:, :], in0=k_r[:, :], in1=Et[:, :], op=ALU.mult), reads=[qkb], writes=[Etb])
                    S.op("dve", lambda v: v.memset(state[:], 0.0), writes=[stateb])
                    order = list(range(NCK)) if dr == 0 else [1, 0] + list(range(NCK - 1, 1, -1))
                    mask = maskf if dr == 0 else maskb
                    def make_chunk(idx, n, dr=dr, mask=mask):
                        sl = slice(n * P, (n + 1) * P)
                        u = idx % 3
                        hold = {}
                        last = idx == NCK - 1

                        def c1():
                            pss, pssb = PS.get()
                            S.op("pe", lambda pe: pe.matmul(pss[:, 0:P], lhsT=ki[:, sl], rhs=qd[:, sl], start=True, stop=True), reads=[dkb], writes=[pssb])
                            S.op("dve", lambda v: v.tensor_tensor(out=sTm[u][:, :], in0=pss[:, 0:P], in1=mask[:, :], op=ALU.mult),
                                 reads=[pssb, cst], writes=[sTmb[u]])
                            if not last:
                                pt, ptb = PS.get()
                                S.op("pe", lambda pe: pe.transpose(out=pt[:, 0:P], in_=ke[:, sl], identity=ident[:]), reads=[Etb, identb], writes=[ptb])
                                S.op("act", lambda a: a.activation(out=keT[u][:, :], in_=pt[:, 0:P], func=AF.Copy), reads=[ptb], writes=[keTb[u]])

                        def c2_():
                            po, pob = PS.get()
                            hold["po"] = (po, pob)
                            for c2 in range(2):
                                S.op("pe", lambda pe: pe.matmul(po[:, c2 * P:(c2 + 1) * P], lhsT=Vh[:, n, c2 * P:(c2 + 1) * P], rhs=sTm[u][:, :],
                                                                start=(c2 == 0), stop=(idx == 0 and c2 == 1)), reads=[Vb, sTmb[u]], writes=[pob])
                            if not last:
                                pkv, pkvb = PSK.get()
                                hold["pkv"] = (pkv, pkvb)
                                S.op("pe", lambda pe: pe.matmul(pkv[:, 0:256], lhsT=keT[u][:, :], rhs=Vh[:, n, :], start=True, stop=True),
                                     reads=[keTb[u], Vb], writes=[pkvb])

                        def c3():
                            po, pob = hold["po"]
                            if idx > 0:
                                for c2 in range(2):
                                    S.op("pe", lambda pe: pe.matmul(po[:, c2 * P:(c2 + 1) * P], lhsT=stbf[:, c2 * P:(c2 + 1) * P], rhs=qd[:, sl],
                                                                    start=False, stop=(c2 == 1)), reads=[stbfb, dkb], writes=[pob])
                            src_ = po[:, 0:256].rearrange("p (c i) -> p c i", c=2)
                            if dr == 0:
                                S.op("act", lambda a: a.activation(out=OT[:, :, sl], in_=src_, func=AF.Copy), reads=[pob], writes=[OTb])
                            else:
                                S.op("dve", lambda v: v.tensor_tensor(out=OT[:, :, sl], in0=src_, in1=OT[:, :, sl], op=ALU.add), reads=[pob], writes=[OTb])

                        def c4():
                            if last:
                                return
                            pkv, pkvb = hold["pkv"]
                            S.op("dve", lambda v: v.scalar_tensor_tensor(out=state[:, :], in0=state[:, :], scalar=cend[:, 1, n:n + 1], in1=pkv[:, 0:256],
                                                                         op0=ALU.mult, op1=ALU.add), reads=[pkvb, cendb], writes=[stateb])
                            S.op("act", lambda a: a.activation(out=stbf[:, :], in_=state[:, :], func=AF.Copy), reads=[stateb], writes=[stbfb])
                        return c1, c2_, c3, c4

                    chunks = [make_chunk(idx, n) for idx, n in enumerate(order)]
                    for step in range(NCK + 3):
                        if 0 <= step - 3 < NCK:
                            chunks[step - 3][3]()
                        if step < NCK:
                            chunks[step][0]()
                        if 0 <= step - 1 < NCK:
                            chunks[step - 1][1]()
                        if 0 <= step - 2 < NCK:
                            chunks[step - 2][2]()
                if hd == 0 and getattr(k, "dbg", None):
                    S.dma("sp", lambda q: q.dma_start(out=k.dbg["ot"][:, :, :], in_=OT[:, :, :]), reads=[OTb])
                    S.dma("sp", lambda q: q.dma_start(out=k.dbg["vh"][:, :, :], in_=Vh[:, :, :]), reads=[Vb])
                for bi, (n0, n1) in enumerate(REC_BLOCKS):
                    u = bi % 2
                    w_ = n1 - n0
                    for c2 in range(2):
                        pg_, pgb_ = PS.get()
                        for c in range(NCH):
                            S.op("pe", lambda pe: pe.matmul(pg_[:, 0:w_], lhsT=wqk[:, c, 2 + c2, :], rhs=HT[:, c, n0:n1],
                                                            start=(c == 0), stop=(c == NCH - 1)), reads=[wvgb, HTb], writes=[pgb_])
                        S.op("act", lambda a: a.activation(out=SG[u][:, c2, 0:w_], in_=pg_[:, 0:w_], func=AF.Silu), reads=[pgb_], writes=[SGb[u]])
                    S.op("act", lambda a: a.activation(out=sq[u][:, :, 0:w_], in_=OT[:, :, n0:n1], func=AF.Square), reads=[OTb], writes=[sqb[u]])
                    pr, prb = PS.get()
                    for c2 in range(2):
                        S.op("pe", lambda pe: pe.matmul(pr[:, 0:w_], lhsT=ones[:, :], rhs=sq[u][:, c2, 0:w_], start=(c2 == 0), stop=(c2 == 1)),
                             reads=[cst, sqb[u]], writes=[prb])
                    S.op("dve", lambda v: v.tensor_scalar(rst[u][:, 0:w_], pr[:, 0:w_], 1.0 / 256.0, EPS, op0=ALU.mult, op1=ALU.add),
                         reads=[prb], writes=[rstb[u]])
                    S.op("act", lambda a: a.activation(out=rst[u][:, 0:w_], in_=rst[u][:, 0:w_], func=AF.Sqrt), reads=[rstb[u]], writes=[rstb[u]])
                    S.op("dve", lambda v: v.reciprocal(rst[u][:, 0:w_], rst[u][:, 0:w_]), reads=[rstb[u]], writes=[rstb[u]])
                    for c2 in range(2):
                        S.op("dve", lambda v: v.scalar_tensor_tensor(out=sq[u][:, c2, 0:w_], in0=OT[:, c2, n0:n1], scalar=gg[:, c2:c2 + 1], in1=rst[u][:, 0:w_],
                                                                     op0=ALU.mult, op1=ALU.mult), reads=[OTb, cst, rstb[u]], writes=[sqb[u]])
                        S.op("pool", lambda g: g.tensor_tensor(out=YTc[c2][:, n0:n1], in0=sq[u][:, c2, 0:w_], in1=SG[u][:, c2, 0:w_], op=ALU.mult),
                             reads=[sqb[u], SGb[u]], writes=[YTcb[c2]])
                for c2 in range(2):
                    S.dma("sp", lambda q: q.dma_start(out=k.YTd[hd * 2 + c2, :, :], in_=YTc[c2][:, :]), reads=[YTcb[c2]])
        S.barrier()
        with contextlib.ExitStack() as es3:
            wa = sb(nc, es3, "l_wa", [P, 2, 8, P], F32)
            wi_ = sb(nc, es3, "l_wi", [P, 2, 8, P], F32)
            S.dma("sp", lambda q: q.dma_start(out=wa[:], in_=k.lru_w_a[ir].rearrange("r n d e -> d r n e")), writes=[cst])
            S.dma("sp", lambda q: q.dma_start(out=wi_[:], in_=k.lru_w_i[ir].rearrange("r n d e -> d r n e")), writes=[cst])
            cw = sb(nc, es3, "l_cw", [P, 4, 8], F32)
            cbias = sb(nc, es3, "l_cb", [P, 8], F32)
            ba = sb(nc, es3, "l_ba", [P, 2, 8], F32)
            bi_ = sb(nc, es3, "l_bi", [P, 2, 8], F32)
            lam = sb(nc, es3, "l_lam", [P, 2, 8], F32)
            with nc.allow_non_contiguous_dma(reason="tiny per-feature vectors"):
                S.dma("sp", lambda q: q.dma_start(out=cw[:], in_=k.lru_conv_w[ir].rearrange("j (n p) -> p j n", p=P)), writes=[cst])
                S.dma("sp", lambda q: q.dma_start(out=cbias[:], in_=k.lru_conv_b[ir].rearrange("(n p) -> p n", p=P)), writes=[cst])
                S.dma("sp", lambda q: q.dma_start(out=ba[:], in_=k.lru_b_a[ir].rearrange("r (n p) -> p r n", p=P)), writes=[cst])
                S.dma("sp", lambda q: q.dma_start(out=bi_[:], in_=k.lru_b_i[ir].rearrange("r (n p) -> p r n", p=P)), writes=[cst])
                S.dma("sp", lambda q: q.dma_start(out=lam[:], in_=k.lru_lambda[ir].rearrange("r (n p) -> p r n", p=P)), writes=[cst])
            S.op("act", lambda a: a.activation(out=lam[:], in_=lam[:], func=AF.Exp, scale=-1.0), reads=[cst], writes=[cst])
            S.op("act", lambda a: a.activation(out=lam[:], in_=lam[:], func=AF.Ln, bias=1.0, scale=1.0), reads=[cst], writes=[cst])
            S.op("dve", lambda v: v.tensor_scalar(lam[:], lam[:], -8.0, None, op0=ALU.mult), reads=[cst], writes=[cst])
            wx0 = sb(nc, es3, "l_wx0", [P, NCH, 2, P], BF16)
            wx = [wx0, wx0]
            wxb0 = Buf()
            wxb = [wxb0, wxb0]
            names = ["xr", "xc", "GG", "R0", "I0", "A0", "R1", "I1", "A1", "H0", "H1"]
            T_ = {n_: sb(nc, es3, "l_" + n_, [P, TS], F32) for n_ in names}
            B_ = {n_: Buf() for n_ in names}
            yb = [sb(nc, es3, "l_y%d" % i, [P, TS], BF16) for i in range(1)]
            ybb = [Buf()]
            lam2 = sb(nc, es3, "l_lam2", [P, 2, 8], F32)
            S.op("dve", lambda v: v.tensor_scalar(lam2[:], lam[:], 2.0, None, op0=ALU.mult), reads=[cst], writes=[cst])
            for bk in range(8):
                j = bk % 2

                S.dma("pool", lambda q: q.dma_start(out=wx[j][:, :, 0, :], in_=k.rec_w_in[ir, :, C_LX + bk * P:C_LX + (bk + 1) * P].rearrange("(c p) n -> p c n", p=P)), writes=[wxb[j]])
                S.dma("pool", lambda q: q.dma_start(out=wx[j][:, :, 1, :], in_=k.rec_w_in[ir, :, C_LG + bk * P:C_LG + (bk + 1) * P].rearrange("(c p) n -> p c n", p=P)), writes=[wxb[j]])

                def ev_x(pq, pqb, n0, n1, bi):
                    S.op("act", lambda a: a.activation(out=T_["xr"][:, n0:n1], in_=pq[:, 0:n1 - n0], func=AF.Copy), reads=[pqb], writes=[B_["xr"]])

                def ev_gt(pq, pqb, n0, n1, bi):
                    S.op("act", lambda a: a.activation(out=T_["GG"][:, n0:n1], in_=pq[:, 0:n1 - n0], func=AF.Copy), reads=[pqb], writes=[B_["GG"]])
                proj_fm(S, PS, HT, HTb, wx[j], wxb[j], lambda c: wx[j][:, c, 0, :], ev_x)
                proj_fm(S, PS, HT, HTb, wx[j], wxb[j], lambda c: wx[j][:, c, 1, :], ev_gt)
                GG, xr, xc = T_["GG"], T_["xr"], T_["xc"]
                for (a_, b_) in ((0, TC), (TC, TS)):
                    S.op("dve", lambda v: v.tensor_scalar(xc[:, a_:b_], xr[:, a_:b_], cw[:, 1, bk:bk + 1], cbias[:, bk:bk + 1], op0=ALU.mult, op1=ALU.add),
                         reads=[B_["xr"], cst], writes=[B_["xc"]])
                    S.op("dve", lambda v: v.scalar_tensor_tensor(out=xc[:, a_ + 1:b_], in0=xr[:, a_:b_ - 1], scalar=cw[:, 0, bk:bk + 1], in1=xc[:, a_ + 1:b_],
                                                                 op0=ALU.mult, op1=ALU.add), reads=[B_["xr"], cst], writes=[B_["xc"]])
                    S.op("dve", lambda v: v.scalar_tensor_tensor(out=xc[:, a_:b_ - 1], in0=xr[:, a_ + 1:b_], scalar=cw[:, 2, bk:bk + 1], in1=xc[:, a_:b_ - 1],
                                                                 op0=ALU.mult, op1=ALU.add), reads=[B_["xr"], cst], writes=[B_["xc"]])
                    S.op("dve", lambda v: v.scalar_tensor_tensor(out=xc[:, a_:b_ - 2], in0=xr[:, a_ + 2:b_], scalar=cw[:, 3, bk:bk + 1], in1=xc[:, a_:b_ - 2],
                                                                 op0=ALU.mult, op1=ALU.add), reads=[B_["xr"], cst], writes=[B_["xc"]])
                for bi, (n0, n1) in enumerate(REC_BLOCKS):
                    for dr in range(2):
                        R_, I_ = T_["R%d" % dr], T_["I%d" % dr]
                        pr, prb = PS.get()
                        S.op("pe", lambda pe: pe.matmul(pr[:, 0:n1 - n0], lhsT=wa[:, dr, bk, :], rhs=xc[:, n0:n1], start=True, stop=True),
                             reads=[cst, B_["xc"]], writes=[prb])
                        S.op("act", lambda a: a.activation(out=R_[:, n0:n1], in_=pr[:, 0:n1 - n0], func=AF.Sigmoid, bias=ba[:, dr, bk:bk + 1], scale=1.0),
                             reads=[prb, cst], writes=[B_["R%d" % dr]])
                        pi, pib = PS.get()
                        S.op("pe", lambda pe: pe.matmul(pi[:, 0:n1 - n0], lhsT=wi_[:, dr, bk, :], rhs=xc[:, n0:n1], start=True, stop=True),
                             reads=[cst, B_["xc"]], writes=[pib])
                        S.op("act", lambda a: a.activation(out=I_[:, n0:n1], in_=pi[:, 0:n1 - n0], func=AF.Sigmoid, bias=bi_[:, dr, bk:bk + 1], scale=1.0),
                             reads=[pib, cst], writes=[B_["I%d" % dr]])
                S.op("pool", lambda g: g.tensor_tensor(out=xr[:, :], in0=GG[:, :], in1=GG[:, :], op=ALU.mult), reads=[B_["GG"], B_["xc"]], writes=[B_["xr"]])
                S.op("dve", lambda v: v.tensor_scalar(xr[:, :], xr[:, :], 0.044715, 1.0, op0=ALU.mult, op1=ALU.add), writes=[B_["xr"]])
                S.op("pool", lambda g: g.tensor_tensor(out=xr[:, :], in0=xr[:, :], in1=GG[:, :], op=ALU.mult), reads=[B_["GG"]], writes=[B_["xr"]])
                S.op("act", lambda a: a.activation(out=xr[:, :], in_=xr[:, :], func=AF.Sigmoid, scale=1.5957691216), writes=[B_["xr"]])
                S.op("dve", lambda v: v.tensor_tensor(out=GG[:, :], in0=GG[:, :], in1=xr[:, :], op=ALU.mult), reads=[B_["xr"]], writes=[B_["GG"]])
                for dr in range(2):
                    R_, I_, A_ = T_["R%d" % dr], T_["I%d" % dr], T_["A%d" % dr]
                    Rb, Ib, Ab = B_["R%d" % dr], B_["I%d" % dr], B_["A%d" % dr]
                    S.op("act", lambda a: a.activation(out=A_[:, :], in_=R_[:, :], func=AF.Exp, scale=lam[:, dr, bk:bk + 1]), reads=[Rb, cst], writes=[Ab])
                    S.op("act", lambda a: a.activation(out=R_[:, :], in_=R_[:, :], func=AF.Exp, scale=lam2[:, dr, bk:bk + 1]), reads=[cst], writes=[Rb])
                    S.op("pool", lambda g: g.tensor_tensor(out=I_[:, :], in0=I_[:, :], in1=xc[:, :], op=ALU.mult), reads=[B_["xc"]], writes=[Ib])
                for dr in range(2):
                    R_, I_ = T_["R%d" % dr], T_["I%d" % dr]
                    Rb, Ib = B_["R%d" % dr], B_["I%d" % dr]
                    S.op("act", lambda a: a.activation(out=R_[:, :], in_=R_[:, :], func=AF.Sqrt, scale=-1.0, bias=1.0), writes=[Rb])
                    S.op("dve", lambda v: v.tensor_tensor(out=R_[:, :], in0=R_[:, :], in1=I_[:, :], op=ALU.mult), reads=[Ib], writes=[Rb])
                H0, H1 = T_["H0"], T_["H1"]
                S.op("dve", lambda v: v.tensor_tensor_scan(out=H0[:, :], data0=T_["A0"][:, :], data1=T_["R0"][:, :], initial=0.0, op0=ALU.mult, op1=ALU.add),
                     reads=[B_["A0"], B_["R0"]], writes=[B_["H0"]])
                S.op("dve", lambda v: v.tensor_tensor_scan(out=H1[:, TC - 1::-1], data0=T_["A1"][:, TC - 1::-1], data1=T_["R1"][:, TC - 1::-1], initial=0.0,
                                                           op0=ALU.mult, op1=ALU.add), reads=[B_["A1"], B_["R1"]], writes=[B_["H1"]])
                S.op("dve", lambda v: v.tensor_tensor_scan(out=H1[:, TS - 1:TC - 1:-1], data0=T_["A1"][:, TS - 1:TC - 1:-1], data1=T_["R1"][:, TS - 1:TC - 1:-1],
                                                           initial=H1[:, 0:1], op0=ALU.mult, op1=ALU.add), reads=[B_["A1"], B_["R1"]], writes=[B_["H1"]])
                S.op("pool", lambda g: g.tensor_tensor(out=H0[:, :], in0=H0[:, :], in1=H1[:, :], op=ALU.add), reads=[B_["H1"]], writes=[B_["H0"]])
                j = 0
                S.op("dve", lambda v: v.tensor_tensor(out=yb[j][:, :], in0=H0[:, :], in1=GG[:, :], op=ALU.mult), reads=[B_["H0"], B_["GG"]], writes=[ybb[j]])
                S.dma("sp", lambda q: q.dma_start(out=k.YTd[8 + bk, :, :], in_=yb[j][:, :]), reads=[ybb[j]])
    S.barrier()


def final_phase(k, S):
    nc = k.nc
    with contextlib.ExitStack() as es:
        xts = [sb(nc, es, "fn_xt%d" % i, [P, D], F32) for i in range(3)]
        xbs = [Buf() for _ in range(3)]
        hms = [sb(nc, es, "fn_hm%d" % i, [P, D], F32) for i in range(3)]
        hbs = [Buf() for _ in range(3)]
        sm = [sb(nc, es, "fn_sm%d" % i, [P, 8], F32) for i in range(3)]
        smb = [Buf() for _ in range(3)]
        grow = sb(nc, es, "fn_g", [P, D], F32)
        gb = Buf()
        S.dma("sp", lambda q: q.dma_start(out=grow[:], in_=k.norm_f_g[0:1, :].partition_broadcast(P)), writes=[gb])
        it = 0
        tiles_st = []
        for s in range(NS):
            for t in range(TL // P):
                j = it % 3
                it += 1
                r0 = s * TS + TC + t * P

                def fA(j=j, r0=r0):
                    S.dma("sp", lambda q: q.dma_start(out=xts[j][:, :], in_=k.X[r0:r0 + P, :]), writes=[xbs[j]])
                    S.op("act", lambda a: a.activation(out=hms[j][:, :], in_=xts[j][:, :], func=AF.Square, accum_out=sm[j][:, 0:1]),
                         reads=[xbs[j]], writes=[hbs[j], smb[j]])

                def fB(j=j):
                    small = sm[j]
                    S.op("dve", lambda v: v.tensor_scalar(small[:, 1:2], small[:, 0:1], 1.0 / D, EPS, op0=ALU.mult, op1=ALU.add),
                         reads=[smb[j]], writes=[smb[j]])
                    S.op("act", lambda a: a.activation(out=small[:, 2:3], in_=small[:, 1:2], func=AF.Sqrt), reads=[smb[j]], writes=[smb[j]])
                    S.op("dve", lambda v: v.reciprocal(small[:, 3:4], small[:, 2:3]), reads=[smb[j]], writes=[smb[j]])
                    S.op("dve", lambda v: v.scalar_tensor_tensor(out=hms[j][:, :], in0=xts[j][:, :], scalar=small[:, 3:4], in1=grow[:, :],
                                                                 op0=ALU.mult, op1=ALU.mult), reads=[xbs[j], smb[j], gb], writes=[hbs[j]])

                def fC(j=j, s=s, t=t):
                    S.dma("pool", lambda q: q.dma_start(out=k.out[s, t * P:(t + 1) * P, :], in_=hms[j][:, :]), reads=[hbs[j]])
                tiles_st.append([fA, fB, fC])
        run_pipelined(tiles_st)
    S.barrier()


WEIGHT_SHAPES = {
    "w_mod": [DEPTH, D, 6 * D], "b_mod": [DEPTH, 6 * D], "norm_mix_g": [DEPTH, D], "norm_ffn_g": [DEPTH, D],
    "w_out": [DEPTH, D, D], "router_w": [DEPTH, D, NE], "exp_w_gate": [DEPTH, NE, D, FF], "exp_w_up": [DEPTH, NE, D, FF],
    "exp_w_down": [DEPTH, NE, FF, D], "rec_w_in": [2, D, 5152], "gla_alpha_up": [2, 2, 16, 512], "gla_alpha_b": [2, 2, 512],
    "gla_norm_g": [2, 256], "lru_conv_w": [2, 4, 1024], "lru_conv_b": [2, 1024], "lru_w_a": [2, 2, 8, P, P],
    "lru_b_a": [2, 2, 1024], "lru_w_i": [2, 2, 8, P, P], "lru_b_i": [2, 2, 1024], "lru_lambda": [2, 2, 1024],
    "na_w_qkv": [2, D, 3 * D], "norm_f_g": [1, D],
}
CONST_SHAPES = {"ident_d": [P, P], "mask_f": [P, P], "mask_b": [P, P], "rope_cos": [P, TL], "rope_sin": [P, TL],
                "na_tab": [2, 16, 5, P, 640]}


def build_program():
    nc = bass.Bass("TRN2", target_bir_lowering=False)
    k = Ctx()
    k.nc = nc

    def inp(name, shape):
        return nc.dram_tensor(name, shape, F32, kind="ExternalInput").ap()
    k.x_in = inp("x", [NS, TL, D])
    k.ctx_in = inp("ctx", [NS, TC, D])
    k.c = inp("c", [NS, D])
    k.c_ctx = inp("c_ctx", [1, D])
    for n_, sh in WEIGHT_SHAPES.items():
        setattr(k, n_, inp(n_, sh))
    for n_, sh in CONST_SHAPES.items():
        setattr(k, n_, inp(n_, sh))
    k.out = nc.dram_tensor("out", [NS, TL, D], F32, kind="ExternalOutput").ap()
    k.X = nc.dram_tensor("scr_X", [NS * TS, D], F32, kind="Internal").ap()
    k.H2 = nc.dram_tensor("scr_H2", [NS * TS, D], F32, kind="Internal").ap()
    k.MODR = nc.dram_tensor("scr_MODR", [DEPTH, 3, 6, D], F32, kind="Internal").ap()
    k.YTd = nc.dram_tensor("scr_YT", [NCH, P, TS], BF16, kind="Internal").ap()
    with contextlib.ExitStack() as es:
        S = Sched(nc, es)
        xb = Buf()
        for s in range(NS):
            S.dma("sp", lambda q: q.dma_start(out=k.X[s * TS:s * TS + TC, :], in_=k.ctx_in[s, :, :]), writes=[xb])
            for hh in range(4):
                S.dma("sp", lambda q: q.dma_start(out=k.X[s * TS + TC + hh * 512:s * TS + TC + (hh + 1) * 512, :],
                                                  in_=k.x_in[s, hh * 512:(hh + 1) * 512, :]), writes=[xb])
        S.barrier()
        mod_phase(k, S)
        for l in range(DEPTH):
            last = l == DEPTH - 1
            for s in range(NS):
                if l % 2 == 0:
                    rec_phase(k, S, l, s)
                else:
                    na_phase(k, S, l, s, do_ctx=not last)
                outproj_phase(k, S, l, s, do_ctx=not last)
            ffn_phase(k, S, l, do_ctx=not last)
        final_phase(k, S)
        S.barrier()
    return nc


def kernel(**inputs):
    n_cores = 8
    f32 = lambda a: np.ascontiguousarray(np.asarray(a, dtype=np.float32))
    shared = {}
    for n_, sh in WEIGHT_SHAPES.items():
        shared[n_] = f32(inputs[n_]).reshape(sh)
    cosF, sinF = build_rope_tabs()
    shared["ident_d"] = np.eye(P, dtype=np.float32)
    shared["mask_f"] = np.triu(np.ones((P, P), np.float32))
    shared["mask_b"] = np.tril(np.ones((P, P), np.float32))
    shared["rope_cos"] = cosF
    shared["rope_sin"] = sinF
    shared["na_tab"] = build_na_tab(f32(inputs["na_rpb"]))
    shared["c_ctx"] = f32(inputs["c_ctx"]).reshape(1, D)
    x = f32(inputs["x"])
    ctx = f32(inputs["ctx"])
    c = f32(inputs["c"])
    in_maps = []
    for i in range(n_cores):
        m = dict(shared)
        m["x"] = x[i * NS:(i + 1) * NS]
        m["ctx"] = ctx[i * NS:(i + 1) * NS]
        m["c"] = c[i * NS:(i + 1) * NS]
        in_maps.append(m)
    nc = build_program()
    res = run_bass_kernel_spmd(nc, in_maps, core_ids=list(range(n_cores)))
    return np.concatenate([np.asarray(r["out"], dtype=np.float32) for r in res.results], axis=0)
```
